# Optimizing a Trainium2 kernel written in Bass

```python
import jax, jax.numpy as jnp
from jax import lax
import numpy as np

D_MODEL = 2048
BATCH = 2
SEQ = 4096
DEPTH = 1

CHUNK = 64
D_MIX = D_MODEL
D_CONV = D_MIX // 2
CONV_GROUPS = 16
D_RWKV = D_MIX - D_CONV
RWKV_HEAD = 64
N_RWKV_HEADS = D_RWKV // RWKV_HEAD
CONV_WIDTH = 31
D_DECAY_LORA = 64
D_AAA_LORA = 64
D_GATE_LORA = 160
N_GROUPS = 4
EXPERTS_PER_GROUP = 8
N_EXPERTS = N_GROUPS * EXPERTS_PER_GROUP
TOP_K = 2
D_EXPERT = 512
MOE_BLOCK = 128
RMS_EPS = 1e-6
LN_EPS = 1e-5
GN_EPS = 64e-5
D_IN = 2 * D_CONV + 3 * D_RWKV + D_DECAY_LORA + D_AAA_LORA + D_GATE_LORA
D_SHIFT = D_IN - 2 * D_CONV

kernel_name = 'hybrid_conformer_rwkv7_hmoe_block'


def rms_norm(x, g):
    xf = x.astype(jnp.float32)
    y = xf * lax.rsqrt(jnp.mean(xf * xf, axis=-1, keepdims=True) + RMS_EPS)
    return (y * g.astype(jnp.float32)).astype(x.dtype)


def layer_norm(u, g, b):
    uf = u.astype(jnp.float32)
    mu = jnp.mean(uf, axis=-1, keepdims=True)
    var = jnp.mean(jnp.square(uf - mu), axis=-1, keepdims=True)
    y = (uf - mu) * lax.rsqrt(var + LN_EPS) * g.astype(jnp.float32) + b.astype(jnp.float32)
    return y.astype(u.dtype)


def prev_frame(u):
    return jnp.pad(u, ((0, 0), (1, 0), (0, 0)))[:, :-1]


def conformer_conv(val, gate, dw, bias, ln_g, ln_b):
    u = val * jax.nn.sigmoid(gate)
    u = lax.conv_general_dilated(
        u, dw[:, None, :].astype(u.dtype), window_strides=(1,),
        padding=[(CONV_WIDTH - 1, 0)], dimension_numbers=('NWC', 'WIO', 'NWC'),
        feature_group_count=D_CONV) + bias
    u = layer_norm(u, ln_g, ln_b)
    return jax.nn.silu(u)


def rwkv7_scan(r, decay, k, v, a, b):
    B, T, H, N = r.shape
    def step(S, inp):
        rt, wt, kt, vt, at, bt = inp
        sa = jnp.einsum('bhvk,bhk->bhv', S, at)
        S = S * wt[:, :, None, :] + sa[..., None] * bt[:, :, None, :] + vt[..., None] * kt[:, :, None, :]
        return S, jnp.einsum('bhvk,bhk->bhv', S, rt)
    S0 = jnp.zeros((B, H, N, N), jnp.float32)
    xs = tuple(jnp.moveaxis(t, 1, 0) for t in (r, decay, k, v, a, b))
    _, y = lax.scan(step, S0, xs)
    return jnp.moveaxis(y, 0, 1)


def rwkv7_time_mix(proj, mu, w0, w_up, a0, a_up, g_up, k_k, k_a, r_k, gn_g, gn_b):
    B, T, _ = proj.shape
    f32 = jnp.float32
    proj = proj + (prev_frame(proj) - proj) * mu
    o1, o2, o3 = D_RWKV, 2 * D_RWKV, 3 * D_RWKV
    r, k, v, wd, ad, gd = jnp.split(
        proj, [o1, o2, o3, o3 + D_DECAY_LORA, o3 + D_DECAY_LORA + D_AAA_LORA], axis=-1)
    w = -jax.nn.softplus(-(w0 + jnp.tanh(wd) @ w_up)) - 0.5
    decay = jnp.exp(-jnp.exp(w.astype(f32)))
    a = jax.nn.sigmoid(a0 + ad @ a_up).astype(f32)
    g = jax.nn.sigmoid(gd) @ g_up
    heads = lambda t: t.astype(f32).reshape(B, T, N_RWKV_HEADS, RWKV_HEAD)
    kk = heads(k * k_k)
    kk = kk / jnp.maximum(jnp.sqrt(jnp.sum(kk * kk, axis=-1, keepdims=True)), 1e-12)
    k = k.astype(f32) * (1.0 + (a - 1.0) * k_a.astype(f32))
    rh, kh, vh, ah, wh = heads(r), heads(k), heads(v), heads(a), heads(decay)
    y = rwkv7_scan(rh, wh, kh, vh, -kk, kk * ah)
    mu_y = jnp.mean(y, axis=-1, keepdims=True)
    var_y = jnp.mean(jnp.square(y - mu_y), axis=-1, keepdims=True)
    y = ((y - mu_y) * lax.rsqrt(var_y + GN_EPS)).reshape(B, T, D_RWKV)
    y = y * gn_g.astype(f32) + gn_b.astype(f32)
    bonus = jnp.sum(rh * kh * r_k.astype(f32), axis=-1, keepdims=True) * vh
    y = y + bonus.reshape(B, T, D_RWKV)
    return (y * g.astype(f32)).astype(proj.dtype)


def hierarchical_moe(h, rg_w, rg_b, re_w, re_b, w_gate, w_up, w_down):
    B, T, D = h.shape
    xt = h.reshape(-1, D)
    NT = xt.shape[0]
    xf = xt.astype(jnp.float32)
    g_logits = xf @ rg_w.astype(jnp.float32) + rg_b.astype(jnp.float32)
    g_prob = jax.nn.softmax(g_logits, axis=-1)
    grp = jnp.argmax(g_logits, axis=-1)
    p_grp = jnp.take_along_axis(g_prob, grp[:, None], axis=-1)
    e_logits = (xf @ re_w.astype(jnp.float32) + re_b.astype(jnp.float32))
    e_logits = e_logits.reshape(NT, N_GROUPS, EXPERTS_PER_GROUP)
    e_logits = jnp.take_along_axis(e_logits, grp[:, None, None], axis=1)[:, 0]
    top_v, top_i = lax.top_k(e_logits, TOP_K)
    gates = p_grp * jax.nn.softmax(top_v, axis=-1)
    expert_id = (grp[:, None] * EXPERTS_PER_GROUP + top_i).astype(jnp.int32)

    A = NT * TOP_K
    flat_e = expert_id.reshape(-1)
    flat_tok = jnp.repeat(jnp.arange(NT, dtype=jnp.int32), TOP_K)
    flat_gate = gates.reshape(-1)
    order = jnp.argsort(flat_e)
    se, stok, sgate = flat_e[order], flat_tok[order], flat_gate[order]
    counts = jnp.zeros((N_EXPERTS,), jnp.int32).at[flat_e].add(1)
    starts = jnp.cumsum(counts) - counts
    padded = (counts + MOE_BLOCK - 1) // MOE_BLOCK * MOE_BLOCK
    pend = jnp.cumsum(padded)
    pstarts = pend - padded
    dest = pstarts[se] + (jnp.arange(A, dtype=jnp.int32) - starts[se])
    n_blocks = (A + N_EXPERTS * (MOE_BLOCK - 1) + MOE_BLOCK - 1) // MOE_BLOCK
    P = n_blocks * MOE_BLOCK
    slot_tok = jnp.full((P,), NT, jnp.int32).at[dest].set(stok)
    slot_gate = jnp.zeros((P,), jnp.float32).at[dest].set(sgate)
    block_e = jnp.minimum(
        jnp.searchsorted(pend, jnp.arange(n_blocks, dtype=jnp.int32) * MOE_BLOCK, side='right'),
        N_EXPERTS - 1)
    x_pad = jnp.concatenate([xt, jnp.zeros((1, D), xt.dtype)], axis=0)
    xb = x_pad[slot_tok].reshape(n_blocks, MOE_BLOCK, D)

    def expert_block(args):
        xblk, e = args
        hid = jax.nn.silu(xblk @ w_gate[e]) * (xblk @ w_up[e])
        return hid @ w_down[e]

    yb = lax.map(expert_block, (xb, block_e))
    y = yb.reshape(P, D) * slot_gate[:, None].astype(yb.dtype)
    out = jnp.zeros((NT + 1, D), yb.dtype).at[slot_tok].add(y)[:NT]
    return out.reshape(B, T, D).astype(h.dtype)


def setup_inputs(seed: int = 0) -> dict:
    key = jax.random.key(seed)
    ks = jax.random.split(key, 28)
    f32 = jnp.float32
    L = DEPTH
    def nrm(k, shape, scale):
        return jax.random.normal(k, shape, f32) * scale
    return {
        'x': nrm(ks[0], (BATCH, SEQ, D_MODEL), 1.0),
        'norm_mix': 1.0 + nrm(ks[1], (L, D_MODEL), 0.02),
        'w_in': nrm(ks[2], (L, D_MODEL, D_IN), D_MODEL ** -0.5),
        'conv_dw': nrm(ks[3], (L, CONV_WIDTH, D_CONV), CONV_WIDTH ** -0.5),
        'conv_b': nrm(ks[4], (L, D_CONV), 0.02),
        'conv_ln_g': 1.0 + nrm(ks[5], (L, D_CONV), 0.02),
        'conv_ln_b': nrm(ks[6], (L, D_CONV), 0.02),
        'shift_mu': jax.random.uniform(ks[7], (L, D_SHIFT), f32),
        'w0': -1.0 + nrm(ks[8], (L, D_RWKV), 0.5),
        'w_lora_up': nrm(ks[9], (L, D_DECAY_LORA, D_RWKV), 0.1 * D_DECAY_LORA ** -0.5),
        'a0': nrm(ks[10], (L, D_RWKV), 0.5),
        'a_lora_up': nrm(ks[11], (L, D_AAA_LORA, D_RWKV), 0.1 * D_AAA_LORA ** -0.5),
        'g_lora_up': nrm(ks[12], (L, D_GATE_LORA, D_RWKV), D_GATE_LORA ** -0.5),
        'k_k': 0.85 + nrm(ks[13], (L, D_RWKV), 0.05),
        'k_a': 1.0 + nrm(ks[14], (L, D_RWKV), 0.05),
        'r_k': nrm(ks[15], (L, N_RWKV_HEADS, RWKV_HEAD), 0.1),
        'gn_g': 1.0 + nrm(ks[16], (L, D_RWKV), 0.02),
        'gn_b': nrm(ks[17], (L, D_RWKV), 0.02),
        'w_out': nrm(ks[18], (L, D_MIX, D_MODEL), D_MIX ** -0.5),
        'norm_ffn': 1.0 + nrm(ks[19], (L, D_MODEL), 0.02),
        'router_group_w': nrm(ks[20], (L, D_MODEL, N_GROUPS), D_MODEL ** -0.5),
        'router_group_b': nrm(ks[21], (L, N_GROUPS), 0.01),
        'router_expert_w': nrm(ks[22], (L, D_MODEL, N_EXPERTS), D_MODEL ** -0.5),
        'router_expert_b': nrm(ks[23], (L, N_EXPERTS), 0.01),
        'expert_w_gate': nrm(ks[24], (L, N_EXPERTS, D_MODEL, D_EXPERT), D_MODEL ** -0.5),
        'expert_w_up': nrm(ks[25], (L, N_EXPERTS, D_MODEL, D_EXPERT), D_MODEL ** -0.5),
        'expert_w_down': nrm(ks[26], (L, N_EXPERTS, D_EXPERT, D_MODEL), D_EXPERT ** -0.5),
        'norm_final': 1.0 + nrm(ks[27], (D_MODEL,), 0.02),
    }


def reference(x, norm_mix, w_in, conv_dw, conv_b, conv_ln_g, conv_ln_b, shift_mu, w0,
              w_lora_up, a0, a_lora_up, g_lora_up, k_k, k_a, r_k, gn_g, gn_b, w_out,
              norm_ffn, router_group_w, router_group_b, router_expert_w, router_expert_b,
              expert_w_gate, expert_w_up, expert_w_down, norm_final):
    h = x
    for l in range(DEPTH):
        u = rms_norm(h, norm_mix[l])
        proj = u @ w_in[l]
        conv_val = proj[..., :D_CONV]
        conv_gate = proj[..., D_CONV:2 * D_CONV]
        rwkv_cols = proj[..., 2 * D_CONV:]
        y_conv = conformer_conv(conv_val, conv_gate, conv_dw[l], conv_b[l], conv_ln_g[l], conv_ln_b[l])
        y_rwkv = rwkv7_time_mix(rwkv_cols, shift_mu[l], w0[l], w_lora_up[l], a0[l], a_lora_up[l],
                                g_lora_up[l], k_k[l], k_a[l], r_k[l], gn_g[l], gn_b[l])
        mixed = jnp.concatenate([y_conv, y_rwkv], axis=-1)
        h = h + mixed @ w_out[l]
        v = rms_norm(h, norm_ffn[l])
        h = h + hierarchical_moe(v, router_group_w[l], router_group_b[l], router_expert_w[l],
                                 router_expert_b[l], expert_w_gate[l], expert_w_up[l], expert_w_down[l])
    return rms_norm(h, norm_final)
```

```python
import numpy as np
import concourse.bass as bass
import concourse.mybir as mybir
from concourse.bass_utils import run_bass_kernel_spmd

F32 = mybir.dt.float32
BF16 = mybir.dt.bfloat16
AF = mybir.ActivationFunctionType
ALU = mybir.AluOpType
ENGS = ("tensor", "vector", "scalar", "gpsimd", "sync")

D = 2048
NKT = 16
SEQ = 4096
OWN = 1024
NSEG = 8
SEGN = 512
C = 64
NCH = SEGN // C
NE = 32
DE = 512
DEBUG = False


class Buf:
    __slots__ = ("name", "w", "rs")

    def __init__(self, name):
        self.name = name
        self.w = None
        self.rs = []


class Sched:
    def __init__(self, nc):
        self.nc = nc
        self.rec = {e: [] for e in ENGS}
        self.sem = {}
        self.cnt = {}
        self.seen = {e: {} for e in ENGS}
        for e in ENGS:
            self._mksem(e)

    def _mksem(self, key):
        self.sem[key] = self.nc.alloc_semaphore("s_" + key)
        self.cnt[key] = 0

    def _wait(self, eng, deps):
        for key, n in deps.items():
            if n <= 0 or self.seen[eng].get(key, 0) >= n:
                continue
            if key == "tensor" and eng == "tensor":
                continue
            self.seen[eng][key] = n
            sem = self.sem[key]
            self.rec[eng].append(lambda e, sem=sem, n=n: e.wait_ge(sem, n))

    def _deps(self, reads, writes):
        deps = {}

        def add(kn):
            if kn is not None and deps.get(kn[0], 0) < kn[1]:
                deps[kn[0]] = kn[1]
        for b in reads:
            add(b.w)
        for b in writes:
            add(b.w)
            for r in b.rs:
                add(r)
        return deps

    def _mark(self, me, reads, writes):
        for b in reads:
            b.rs.append(me)
        for b in writes:
            b.w = me
            b.rs = []

    def op(self, eng, fn, reads=(), writes=()):
        self._wait(eng, self._deps(reads, writes))
        self.cnt[eng] += 1
        sem = self.sem[eng]
        self.rec[eng].append(lambda e, fn=fn, sem=sem: fn(e).then_inc(sem, 1))
        self._mark((eng, self.cnt[eng]), reads, writes)

    def dma(self, eng, fn, reads=(), writes=(), key=None):
        if key is None:
            key = "d_" + (writes[0].name if writes else reads[0].name)
        if key not in self.sem:
            self._mksem(key)
        self._wait(eng, self._deps(reads, writes))
        self.cnt[key] += 16
        sem = self.sem[key]
        self.rec[eng].append(lambda e, fn=fn, sem=sem: fn(e).then_inc(sem, 16))
        self._mark((key, self.cnt[key]), reads, writes)

    def barrier(self):
        for e in ENGS:
            self._wait(e, {k: v for k, v in self.cnt.items() if v > 0 and k != e})

    def flush(self, block):
        for e in ENGS:
            lst = self.rec[e]
            if not lst:
                continue
            self.rec[e] = []

            def body(engine, lst=lst):
                for f in lst:
                    f(engine)
            getattr(block, e)(body)


def _ptab_cols():
    cols = {}
    n = 0
    for ct in range(8):
        for nm in ("mu_r", "mu_k", "mu_v", "w0", "a0", "k_k", "k_a", "r_k", "gn_g", "gn_b"):
            cols[(nm, ct)] = n
            n += 1
    for cc in range(8):
        for nm in ("conv_b", "ln_g", "ln_b"):
            cols[(nm, cc)] = n
            n += 1
        cols[("dw", cc)] = n
        n += 31
    for nm in ("norm_mix", "norm_ffn", "norm_final"):
        cols[nm] = n
        n += 16
    for nm in ("mu_wa", "mu_g1", "mu_g2"):
        cols[nm] = n
        n += 1
    return cols, n


PCOL, NP = _ptab_cols()


def build_nc():
    nc = bass.Bass("TRN2", target_bir_lowering=False)
    dt_in = lambda name, shape, dt=F32: nc.dram_tensor(name, shape, dt, kind="ExternalInput").ap()
    xseq = dt_in("xseq", [SEQ, D])
    ptab_d = dt_in("ptab", [128, NP])
    wct_d = dt_in("wct", [8, 128, NKT, 384])
    wlora_d = dt_in("wlora", [128, NKT, 288])
    wconv_d = dt_in("wconv", [8, 128, NKT, 256])
    waup_d = dt_in("waup", [128, 1024])
    gup1_d = dt_in("gup1", [128, 1024])
    gup2_d = dt_in("gup2", [32, 1024])
    wout_d = dt_in("wout", [4, 128, NKT, 512])
    rw_d = dt_in("rw", [128, NKT, 36])
    rb_d = dt_in("rb", [1, 36])
    wg_d = dt_in("wg", [NE, 128, NKT, DE])
    wu_d = dt_in("wu", [NE, 128, NKT, DE])
    wd_d = dt_in("wd", [NE, 128, 4, D])
    out_d = nc.dram_tensor("out", [OWN, D], F32, kind="ExternalOutput").ap()
    if DEBUG:
        dbg_mix = nc.dram_tensor("dbg_mix", [128, 16, OWN], F32, kind="ExternalOutput").ap()
        dbg_h = nc.dram_tensor("dbg_h", [128, 16, OWN], F32, kind="ExternalOutput").ap()

    S = Sched(nc)
    A = nc.alloc_sbuf_tensor
    bufs = {}

    def B(name):
        if name not in bufs:
            bufs[name] = Buf(name)
        return bufs[name]

    def mm(out, lhsT, rhs, start, stop, r, w):
        S.op("tensor", lambda e: e.matmul(out, lhsT=lhsT, rhs=rhs, start=start, stop=stop), r, w)

    def tr(out, in_, ident, r, w):
        S.op("tensor", lambda e: e.transpose(out=out, in_=in_, identity=ident), r, w)

    def act(out, in_, func, r, w, bias=None, scale=None, accum=None):
        kw = {}
        if bias is not None:
            kw["bias"] = bias
        if scale is not None:
            kw["scale"] = scale
        if accum is not None:
            kw["accum_out"] = accum
        S.op("scalar", lambda e: e.activation(out=out, in_=in_, func=func, **kw), r, w)

    def tt(out, a, b, op, r, w, eng="vector"):
        S.op(eng, lambda e: e.tensor_tensor(out=out, in0=a, in1=b, op=op), r, w)

    def ts(out, a, s1, s2, op0, op1, r, w, eng="vector"):
        S.op(eng, lambda e: e.tensor_scalar(out=out, in0=a, scalar1=s1, scalar2=s2, op0=op0, op1=op1), r, w)

    def stt(out, a, s, b, op0, op1, r, w, eng="vector"):
        S.op(eng, lambda e: e.scalar_tensor_tensor(out=out, in0=a, scalar=s, in1=b, op0=op0, op1=op1), r, w)

    def cp(out, in_, r, w, eng="vector"):
        S.op(eng, lambda e: e.tensor_copy(out=out, in_=in_), r, w)

    def recip(out, in_, r, w):
        S.op("vector", lambda e: e.reciprocal(out=out, in_=in_), r, w)

    def memset(ap, val, w, eng="gpsimd"):
        S.op(eng, lambda e: e.memset(ap, val), (), w)

    def dma(eng, out, in_, r, w, key=None):
        S.dma(eng, lambda e: e.dma_start(out=out, in_=in_), r, w, key)

    identF = A("identF", [128, 128], F32)
    identB = A("identB", [128, 128], BF16)
    ones128 = A("ones128", [128, 128], F32)
    blockones = A("blockones", [128, 128], F32)
    ident2 = A("ident2", [128, 64], F32)
    maskS = A("maskS", [128, 128], F32)
    maskL = A("maskL", [128, 64], F32)
    chunkmask = A("chunkmask", [128, SEGN], F32)
    ptab = A("ptab_sb", [128, NP], F32)
    omka = A("omka", [128, 8], F32)
    epst = A("epst", [128, 4], F32)
    PS = [nc.alloc_psum_tensor(f"ps{i}", [128, 512], F32) for i in range(8)]
    PB = [B(f"ps{i}") for i in range(8)]
    CONST = B("const")

    def pc(name, ct=None, n=1):
        c0 = PCOL[(name, ct)] if ct is not None else PCOL[name]
        return ptab[:, c0:c0 + n]

    psi = [0]

    def nextps():
        i = psi[0]
        psi[0] = (i + 1) % 8
        return PS[i], PB[i]

    with nc.Block() as block:
        dma("sync", ptab[:, :], ptab_d, (), [B("ptab")])
        memset(identF[:, :], 0.0, [CONST])
        S.op("gpsimd", lambda e: e.affine_select(out=identF[:, :], in_=identF[:, :], pattern=[[-1, 128]],
                                                 compare_op=ALU.not_equal, fill=1.0, base=0, channel_multiplier=1),
             [CONST], [CONST])
        cp(identB[:, :], identF[:, :], [CONST], [CONST])
        memset(ones128[:, :], 1.0, [CONST])
        memset(blockones[:, :], 0.0, [CONST])
        memset(blockones[0:64, 0:64], 1.0 / 64.0, [CONST])
        memset(blockones[64:128, 64:128], 1.0 / 64.0, [CONST])
        tt(ident2[:, :], identF[:, 0:64], identF[:, 64:128], ALU.add, [CONST], [CONST])
        memset(maskS[:, :], 1.0, [CONST])
        memset(maskL[:, :], 1.0, [CONST])
        for h in range(2):
            hs = slice(h * 64, (h + 1) * 64)
            S.op("gpsimd", lambda e, hs=hs: e.affine_select(out=maskS[hs, 0:64], in_=maskS[hs, 0:64], pattern=[[1, 64]],
                                                            compare_op=ALU.is_gt, fill=0.0, base=0, channel_multiplier=-1),
                 [CONST], [CONST])
            S.op("gpsimd", lambda e, hs=hs: e.affine_select(out=maskS[hs, 64:128], in_=maskS[hs, 64:128], pattern=[[1, 64]],
                                                            compare_op=ALU.is_ge, fill=0.0, base=0, channel_multiplier=-1),
                 [CONST], [CONST])
            S.op("gpsimd", lambda e, hs=hs: e.affine_select(out=maskL[hs, :], in_=maskL[hs, :], pattern=[[-1, 64]],
                                                            compare_op=ALU.is_gt, fill=0.0, base=0, channel_multiplier=1),
                 [CONST], [CONST])
        memset(chunkmask[:, :], 1.0, [CONST])
        memset(chunkmask[:, :].rearrange("p (c t) -> p c t", t=C)[:, :, 0:1], 0.0, [CONST])
        memset(epst[:, 0:1], 1e-6, [CONST])
        memset(epst[:, 1:2], 1e-5, [CONST])
        memset(epst[:, 2:3], 64e-5, [CONST])
        memset(epst[:, 3:4], 0.0, [CONST])
        for ct in range(8):
            ts(omka[:, ct:ct + 1], pc("k_a", ct), -1.0, 1.0, ALU.mult, ALU.add, [B("ptab"), CONST], [CONST])
        S.barrier()
        S.flush(block)

        import contextlib
        mixscr = nc.dram_tensor("mixscr", [128, NKT, OWN], BF16).ap()
        with contextlib.ExitStack() as es:
            def T(name, shape, dt=F32):
                return es.enter_context(nc.sbuf_tensor(name, shape, dt))
            xrows = [T(f"xrow{i}", [128, D]) for i in range(2)]
            ssq = T("ssq", [128, 2])
            uT = T("uT", [128, NKT, SEGN], BF16)
            wcts = [T(f"wct{i}", [128, NKT, 384], BF16) for i in range(2)]
            wlora = T("wlora_sb", [128, NKT, 288], BF16)
            wconv = T("wconv_sb", [128, NKT, 256], BF16)
            waup = T("waup_sb", [128, 1024], BF16)
            gup1 = T("gup1_sb", [128, 1024], BF16)
            gup2 = T("gup2_sb", [32, 1024], BF16)
            raw = T("raw", [128, SEGN + 1])
            dtmp = T("dtmp", [128, SEGN])
            carry = T("carry", [128, 8, 3])
            lcarry = T("lcarry", [128, 3])
            twad = T("twad", [128, SEGN], BF16)
            sg1 = T("sg1", [128, SEGN], BF16)
            sg2 = T("sg2", [32, SEGN], BF16)
            lsh = T("lsh", [128, SEGN])
            k_sh = T("k_sh", [128, SEGN])
            r_sh = T("r_sh", [128, SEGN])
            ls = T("ls", [128, SEGN])
            asig = T("asig", [128, SEGN])
            cl = T("cl", [128, SEGN])
            t0 = T("t0", [128, SEGN])
            e_neg = T("e_neg", [128, SEGN])
            e_pos = T("e_pos", [128, SEGN])
            e_prev = T("e_prev", [128, SEGN])
            e_hat = T("e_hat", [128, SEGN])
            kk = T("kk", [128, SEGN])
            t1 = T("t1", [128, SEGN])
            kmod = T("kmod", [128, SEGN])
            bvec = T("bvec", [128, SEGN])
            ARs = [T(f"AR{i}", [128, 2, SEGN], BF16) for i in range(2)]
            BKs = [T(f"BK{i}", [128, 2, SEGN], BF16) for i in range(2)]
            BKHs = [T(f"BKH{i}", [128, 2, SEGN], BF16) for i in range(2)]
            vbfs = [T(f"vbf{i}", [128, SEGN], BF16) for i in range(2)]
            DWs = [T(f"DW{i}", [128, NCH, 64]) for i in range(2)]
            vks = [T(f"vk{i}", [128, SEGN]) for i in range(3)]
            rkrs = [T(f"rkr{i}", [128, SEGN]) for i in range(3)]
            gTs = [T(f"gT{i}", [128, SEGN]) for i in range(3)]
            SCb = T("SCb", [128, NCH, 128], BF16)
            SCk = T("SCk", [128, NCH, 128], BF16)
            NN = [T(f"NN{i}", [128, NCH, 64], BF16) for i in range(2)]
            LL = [T(f"LL{i}", [128, NCH, 64], BF16) for i in range(2)]
            G = [T(f"G{i}", [128, NCH, 128], BF16) for i in range(2)]
            Vtok = T("Vtok", [128, NCH, 64], BF16)
            Btok = T("Btok", [128, NCH, 64], BF16)
            Ktok = T("Ktok", [128, NCH, 64], BF16)
            PTs = [T(f"PT{i}", [128, NCH, 64], BF16) for i in range(2)]
            Zs = [T(f"Z{i}", [128, NCH, 64]) for i in range(2)]
            QTs = [T(f"QT{i}", [128, NCH, 64], BF16) for i in range(2)]
            Y0Ts = [T(f"Y0T{i}", [128, NCH, 64]) for i in range(2)]
            STs = T("STs", [128, NCH + 1, 64], BF16)
            STp = T("STp", [128, 8, 64], BF16)
            yT = T("yT", [128, SEGN])
            ybf = T("ybf", [128, SEGN], BF16)
            t2 = T("t2", [128, SEGN])
            rstd_bc = T("rstd_bc", [128, SEGN])
            glu = T("glu", [128, 32 + SEGN], BF16)
            gluc = T("gluc", [128, 8, 32], BF16)
            dg = T("dg", [128, 4, 128], BF16)
            conv_out = T("conv_out", [128, 8, SEGN], BF16)
            sq_junk = conv_out[:, 0:4, :]
            onesB = T("onesB", [128, 128], BF16)
            uhalo = T("uhalo", [128, NKT, 32], BF16)
            cbf = ybf
            cmean, crstd, ct1 = e_neg, e_pos, t1

            dma("gpsimd", wlora[:, :, :], wlora_d, (), [B("wlora")])
            dma("gpsimd", waup[:, :], waup_d, (), [B("waup")])
            dma("gpsimd", gup1[:, :], gup1_d, (), [B("gup1")])
            dma("gpsimd", gup2[:, :], gup2_d, (), [B("gup2")])
            memset(carry[:, :, :], 0.0, [B("carry")])
            memset(lcarry[:, :], 0.0, [B("lcarry")])
            memset(STp[:, :, :], 0.0, [B("STp")])
            memset(gluc[:, :, :], 0.0, [B("gluc")])
            cp(onesB[:, :], ones128[:, :], [CONST], [B("onesB")])

            PSETS = {"X": [0, 1, 2], "Y1": [3, 4, 5], "Y2": [6, 7]}
            pidx = {"X": 0, "Y1": 0, "Y2": 0}

            def nps(stage):
                lst = PSETS[stage]
                i = lst[pidx[stage] % len(lst)]
                pidx[stage] += 1
                return PS[i], PB[i]

            hsl = [slice(0, 64), slice(64, 128)]

            def csl(c):
                return slice(c * C, (c + 1) * C)

            def v3(ap, b):
                return ap.rearrange("p (a b) -> p a b", b=b)

            def shift(ps_ap, np_, carry_ap, mu_ap, out_ap, rb, wb, cb):
                act(raw[0:np_, 1:SEGN + 1], ps_ap, AF.Copy, rb, [B("raw")])
                act(raw[0:np_, 0:1], carry_ap, AF.Copy, [cb, B("raw")], [B("raw")])
                tt(dtmp[0:np_, :], raw[0:np_, 0:SEGN], raw[0:np_, 1:SEGN + 1], ALU.subtract, [B("raw")], [B("dtmp")])
                stt(out_ap, dtmp[0:np_, :], mu_ap, raw[0:np_, 1:SEGN + 1], ALU.mult, ALU.add,
                    [B("dtmp"), B("raw"), B("ptab")], wb)
                act(carry_ap, raw[0:np_, SEGN:SEGN + 1], AF.Copy, [B("raw")], [cb])

            def proj(ps, psb, wtile, wb, c0, m, rhsT, rhsb, ncols=SEGN):
                for kt in range(NKT):
                    mm(ps[0:m, 0:ncols], wtile[:, kt, c0:c0 + m], rhsT[:, kt, 0:ncols], kt == 0, kt == NKT - 1,
                       [wb, rhsb], [psb])

            def conv_glu(rhsT, rhsb, ncols, cc, dst_ap, dstb):
                dma("gpsimd", wconv[:, :, :], wconv_d[cc], (), [B("wconv")])
                pv, pvb = nps("X")
                proj(pv, pvb, wconv, B("wconv"), 0, 128, rhsT, rhsb, ncols)
                pg, pgb = nps("X")
                proj(pg, pgb, wconv, B("wconv"), 128, 128, rhsT, rhsb, ncols)
                act(ct1[:, 0:ncols], pg[:, 0:ncols], AF.Sigmoid, [pgb], [B("t1")])
                tt(dst_ap, pv[:, 0:ncols], ct1[:, 0:ncols], ALU.mult, [pvb, B("t1")], dstb)

            def prologue(seg):
                def xload(g):
                    if g < NSEG * 4:
                        dma("sync", xrows[g % 2][:, :], xseq[g * 128:(g + 1) * 128, :], (), [B(f"xrow{g % 2}")])
                if seg == 0:
                    xload(0)
                    xload(1)
                for rt in range(4):
                    g_ = seg * 4 + rt
                    xrow, xb = xrows[g_ % 2], B(f"xrow{g_ % 2}")
                    act(sq_junk, xrow[:, :].rearrange("p (a b) -> p a b", b=SEGN), AF.Square, [xb], [B("conv_all"), B("ssq")], accum=ssq[:, 0:1])
                    act(ssq[:, 1:2], ssq[:, 0:1], AF.Sqrt, [B("ssq"), CONST], [B("ssq")], bias=epst[:, 0:1], scale=1.0 / D)
                    recip(ssq[:, 1:2], ssq[:, 1:2], [B("ssq")], [B("ssq")])
                    ts(xrow[:, :], xrow[:, :], ssq[:, 1:2], None, ALU.mult, ALU.bypass, [xb, B("ssq")], [xb])
                    yield
                    for q in range(4):
                        ps, psb = nps("X")
                        for i in range(4):
                            kt = q * 4 + i
                            tr(ps[:, i * 128:(i + 1) * 128], xrow[:, kt * 128:(kt + 1) * 128], identF[:, :], [xb, CONST], [psb])
                        gcol = PCOL["norm_mix"] + q * 4
                        tt(uT[:, q * 4:q * 4 + 4, rt * 128:(rt + 1) * 128], v3(ps[:, :], 128),
                           ptab[:, gcol:gcol + 4].unsqueeze(2).to_broadcast([128, 4, 128]),
                           ALU.mult, [psb, B("ptab")], [B("uT")])
                        yield
                    xload(g_ + 2)
                ps, psb = nps("X")
                proj(ps, psb, wlora, B("wlora"), 0, 128, uT, B("uT"))
                shift(ps[:, :], 128, lcarry[:, 0:1], pc("mu_wa"), lsh[:, :], [psb], [B("lsh")], B("lcarry"))
                act(twad[0:64, :], lsh[0:64, :], AF.Tanh, [B("lsh")], [B("twad")])
                act(twad[64:128, :], lsh[64:128, :], AF.Copy, [B("lsh")], [B("twad")])
                yield
                ps, psb = nps("X")
                proj(ps, psb, wlora, B("wlora"), 128, 128, uT, B("uT"))
                shift(ps[:, :], 128, lcarry[:, 1:2], pc("mu_g1"), lsh[:, :], [psb], [B("lsh")], B("lcarry"))
                act(sg1[:, :], lsh[:, :], AF.Sigmoid, [B("lsh")], [B("sg1")])
                yield
                ps, psb = nps("X")
                proj(ps, psb, wlora, B("wlora"), 256, 32, uT, B("uT"))
                shift(ps[0:32, :], 32, lcarry[0:32, 2:3], pc("mu_g2")[0:32, :], lsh[0:32, :], [psb], [B("lsh")], B("lcarry"))
                act(sg2[:, :], lsh[0:32, :], AF.Sigmoid, [B("lsh")], [B("sg2")])
                yield

            def conv_branch(seg, own, osg):
                if seg == NSEG - 3:
                    cp(uhalo[:, :, :], uT[:, :, SEGN - 32:SEGN], [B("uT")], [B("uhalo")], eng="gpsimd")
                    for cc in range(8):
                        conv_glu(uhalo, B("uhalo"), 32, cc, gluc[:, cc, :], [B("gluc")])
                        yield
                if not own:
                    return
                for cc in range(8):
                    gb = B("glu")
                    cp(glu[:, 0:32], gluc[:, cc, :], [B("gluc")], [gb], eng="gpsimd")
                    conv_glu(uT, B("uT"), SEGN, cc, glu[:, 32:32 + SEGN], [gb])
                    cp(gluc[:, cc, :], glu[:, SEGN:SEGN + 32], [gb], [B("gluc")], eng="gpsimd")
                    yield
                    dwc = PCOL[("dw", cc)]
                    cob = B("conv_all")
                    ps, psb = nps("X")
                    for j in range(31):
                        dgb = B(f"dg{j % 4}")
                        ts(dg[:, j % 4, :], identB[:, :], ptab[:, dwc + j:dwc + j + 1], None, ALU.mult, ALU.bypass,
                           [CONST, B("ptab")], [dgb])
                        mm(ps[:, :], dg[:, j % 4, :], glu[:, 2 + j:2 + j + SEGN], j == 0, j == 30, [dgb, gb], [psb])
                        if j % 8 == 7:
                            yield
                    act(conv_out[:, cc, :], ps[:, :], AF.Identity, [psb, B("ptab")], [cob], bias=pc("conv_b", cc))
                    yield
                cob = B("conv_all")
                ps, psb = nps("X")
                for cc in range(8):
                    mm(ps[:, :], onesB[:, :], conv_out[:, cc, :], cc == 0, cc == 7, [B("onesB"), cob], [psb])
                act(cmean[:, :], ps[:, :], AF.Copy, [psb], [B("e_neg")], scale=1.0 / 1024.0)
                yield
                ps, psb = nps("X")
                for cc in range(8):
                    tt(kk[:, :], conv_out[:, cc, :], cmean[:, :], ALU.subtract, [cob, B("e_neg")], [B("kk")])
                    act(ct1[:, :], kk[:, :], AF.Square, [B("kk")], [B("t1")])
                    mm(ps[:, :], ones128[:, :], ct1[:, :], cc == 0, cc == 7, [CONST, B("t1")], [psb])
                    yield
                act(crstd[:, :], ps[:, :], AF.Sqrt, [psb, CONST], [B("e_pos")], bias=epst[:, 1:2], scale=1.0 / 1024.0)
                recip(crstd[:, :], crstd[:, :], [B("e_pos")], [B("e_pos")])
                for cc in range(8):
                    tt(kk[:, :], conv_out[:, cc, :], cmean[:, :], ALU.subtract, [cob, B("e_neg")], [B("kk")])
                    tt(ct1[:, :], kk[:, :], crstd[:, :], ALU.mult, [B("kk"), B("e_pos")], [B("t1")])
                    act(cbf[:, :], ct1[:, :], AF.Silu, [B("t1"), B("ptab")], [B("ybf")],
                        bias=pc("ln_b", cc), scale=pc("ln_g", cc))
                    dma("sync", mixscr[:, cc, osg * SEGN:(osg + 1) * SEGN], cbf[:, :], [B("ybf")], ())
                    yield

            K0 = 0.6065306597126334

            def stageX(it):
                seg, ct = divmod(it, 8)
                own = seg >= NSEG - 2
                i2, i3 = it % 2, it % 3
                AR, BK, BKH, vbf, DW = ARs[i2], BKs[i2], BKHs[i2], vbfs[i2], DWs[i2]
                ARb, BKb, BKHb, vbfb, DWb = B(f"AR{i2}"), B(f"BK{i2}"), B(f"BKH{i2}"), B(f"vbf{i2}"), B(f"DW{i2}")
                v_sh, rkr, gT = vks[i3], rkrs[i3], gTs[i3]
                vkb, rkrb, gTb = B(f"vk{i3}"), B(f"rkr{i3}"), B(f"gT{i3}")
                if ct == 0:
                    yield from prologue(seg)
                wct, wtb = wcts[it % 2], B(f"wct{it % 2}")
                if it == 0:
                    dma("gpsimd", wct[:, :, :], wct_d[ct], (), [wtb])
                if it + 1 < NSEG * 8:
                    dma("gpsimd", wcts[(it + 1) % 2][:, :, :], wct_d[(ct + 1) % 8], (), [B(f"wct{(it + 1) % 2}")])
                cols = slice(ct * 128, (ct + 1) * 128)
                ps, psb = nps("X")
                proj(ps, psb, wct, wtb, 0, 128, uT, B("uT"))
                shift(ps[:, :], 128, carry[:, ct, 0:1], pc("mu_k", ct), k_sh[:, :], [psb], [B("k_sh")], B("carry"))
                yield
                ps, psb = nps("X")
                proj(ps, psb, wct, wtb, 128, 128, uT, B("uT"))
                shift(ps[:, :], 128, carry[:, ct, 1:2], pc("mu_v", ct), v_sh[:, :], [psb], [vkb], B("carry"))
                act(vbf[:, :], v_sh[:, :], AF.Copy, [vkb], [vbfb])
                yield
                if own:
                    ps, psb = nps("X")
                    proj(ps, psb, wct, wtb, 256, 128, uT, B("uT"))
                    shift(ps[:, :], 128, carry[:, ct, 2:3], pc("mu_r", ct), r_sh[:, :], [psb], [B("r_sh")], B("carry"))
                    yield
                ps, psb = nps("X")
                mm(ps[:, :], waup[0:64, cols], twad[0:64, :], True, True, [B("waup"), B("twad")], [psb])
                act(ls[:, :], ps[:, :], AF.Sigmoid, [psb, B("ptab")], [B("ls")], bias=pc("w0", ct))
                ps, psb = nps("X")
                mm(ps[:, :], waup[64:128, cols], twad[64:128, :], True, True, [B("waup"), B("twad")], [psb])
                act(asig[:, :], ps[:, :], AF.Sigmoid, [psb, B("ptab")], [B("asig")], bias=pc("a0", ct))
                yield
                S.op("vector", lambda e: e.tensor_tensor_scan(out=cl[:, :], data0=chunkmask[:, :], data1=ls[:, :], initial=0.0,
                                                              op0=ALU.mult, op1=ALU.add),
                     [CONST, B("ls")], [B("cl")])
                act(e_neg[:, :], cl[:, :], AF.Exp, [B("cl")], [B("e_neg")], scale=K0)
                act(e_pos[:, :], cl[:, :], AF.Exp, [B("cl")], [B("e_pos")], scale=-K0)
                tt(t0[:, :], cl[:, :], ls[:, :], ALU.subtract, [B("cl"), B("ls")], [B("t0")], eng="gpsimd")
                yield
                act(e_prev[:, :], t0[:, :], AF.Exp, [B("t0")], [B("e_prev")], scale=-K0)
                cl3 = v3(cl[:, :], C)
                tt(v3(t0[:, :], C), cl3[:, :, C - 1:C].to_broadcast([128, NCH, C]), cl3,
                   ALU.subtract, [B("cl"), B("e_prev")], [B("t0")], eng="gpsimd")
                act(e_hat[:, :], t0[:, :], AF.Exp, [B("t0")], [B("e_hat")], scale=-K0)
                ep3 = v3(e_pos[:, :], C)
                tt(DW[:, :, :], ident2[:, :].unsqueeze(1).to_broadcast([128, NCH, 64]),
                   ep3[:, :, C - 1:C].to_broadcast([128, NCH, 64]), ALU.mult, [CONST, B("e_pos")], [DWb], eng="gpsimd")
                yield
                ts(kk[:, :], k_sh[:, :], pc("k_k", ct), None, ALU.mult, ALU.bypass, [B("k_sh"), B("ptab")], [B("kk")])
                act(t1[:, :], kk[:, :], AF.Square, [B("kk")], [B("t1")])
                ps, psb = nps("X")
                mm(ps[:, :], blockones[:, :], t1[:, :], True, True, [CONST, B("t1")], [psb])
                act(t1[:, :], ps[:, :], AF.Sqrt, [psb], [B("t1")], scale=64.0)
                yield
                ts(t1[:, :], t1[:, :], 1e-12, None, ALU.max, ALU.bypass, [B("t1")], [B("t1")])
                recip(t1[:, :], t1[:, :], [B("t1")], [B("t1")])
                tt(kk[:, :], kk[:, :], t1[:, :], ALU.mult, [B("kk"), B("t1")], [B("kk")])
                yield
                ts(t1[:, :], asig[:, :], pc("k_a", ct), omka[:, ct:ct + 1], ALU.mult, ALU.add,
                   [B("asig"), B("ptab"), CONST], [B("t1")])
                tt(kmod[:, :], k_sh[:, :], t1[:, :], ALU.mult, [B("k_sh"), B("t1")], [B("kmod")])
                tt(bvec[:, :], kk[:, :], asig[:, :], ALU.mult, [B("kk"), B("asig")], [B("bvec")], eng="gpsimd")
                yield
                stt(AR[:, 0, :], kk[:, :], -1.0, e_prev[:, :], ALU.mult, ALU.mult, [B("kk"), B("e_prev")], [ARb])
                tt(BK[:, 0, :], bvec[:, :], e_neg[:, :], ALU.mult, [B("bvec"), B("e_neg")], [BKb], eng="gpsimd")
                tt(BK[:, 1, :], kmod[:, :], e_neg[:, :], ALU.mult, [B("kmod"), B("e_neg")], [BKb])
                yield
                tt(BKH[:, 0, :], bvec[:, :], e_hat[:, :], ALU.mult, [B("bvec"), B("e_hat")], [BKHb], eng="gpsimd")
                tt(BKH[:, 1, :], kmod[:, :], e_hat[:, :], ALU.mult, [B("kmod"), B("e_hat")], [BKHb])
                yield
                if own:
                    tt(AR[:, 1, :], r_sh[:, :], e_pos[:, :], ALU.mult, [B("r_sh"), B("e_pos")], [ARb], eng="gpsimd")
                    stt(rkr[:, :], r_sh[:, :], pc("r_k", ct), kmod[:, :], ALU.mult, ALU.mult,
                        [B("r_sh"), B("kmod"), B("ptab")], [rkrb])
                    ps, psb = nps("X")
                    mm(ps[:, :], gup1[:, cols], sg1[:, :], True, False, [B("gup1"), B("sg1")], [psb])
                    mm(ps[:, :], gup2[:, cols], sg2[:, :], False, True, [B("gup2"), B("sg2")], [psb])
                    act(gT[:, :], ps[:, :], AF.Copy, [psb], [gTb])
                    yield
                if ct == 7:
                    yield from conv_branch(seg, own, seg - (NSEG - 2))

            def stageY1(it):
                seg, ct = divmod(it, 8)
                own = seg >= NSEG - 2
                i2 = it % 2
                AR, BK, BKH, vbf, DW = ARs[i2], BKs[i2], BKHs[i2], vbfs[i2], DWs[i2]
                ARb, BKb, BKHb, vbfb, DWb = B(f"AR{i2}"), B(f"BK{i2}"), B(f"BKH{i2}"), B(f"vbf{i2}"), B(f"DW{i2}")
                PT, Z, QT, Y0T = PTs[i2], Zs[i2], QTs[i2], Y0Ts[i2]
                PTb, Zb, QTb, Y0Tb = B(f"PT{i2}"), B(f"Z{i2}"), B(f"QT{i2}"), B(f"Y0T{i2}")
                ncol = 128 if own else 64
                for which, SCx, SCxb in ((0, SCb, B("SCb")), (1, SCk, B("SCk"))):
                    for half in range(2):
                        ps, psb = nps("Y1")
                        for cc in range(4):
                            c = half * 4 + cc
                            for h in range(2):
                                mm(ps[hsl[h], cc * 128:cc * 128 + ncol], BK[hsl[h], which, csl(c)],
                                   AR[hsl[h], 0:(2 if own else 1), csl(c)], True, True, [BKb, ARb], [psb])
                        tt(SCx[:, half * 4:half * 4 + 4, 0:ncol], v3(ps[:, :], 128)[:, :, 0:ncol],
                           maskS[:, 0:ncol].unsqueeze(1).to_broadcast([128, 4, ncol]), ALU.mult, [psb, CONST], [SCxb])
                        yield
                ps, psb = nps("Y1")
                for c in range(NCH):
                    for h in range(2):
                        mm(ps[hsl[h], c * 64:(c + 1) * 64], AR[hsl[h], 0, csl(c)], BK[hsl[h], 0, csl(c)], True, True, [ARb, BKb], [psb])
                tt(LL[0][:, :, :], v3(ps[:, :], 64), maskL[:, :].unsqueeze(1).to_broadcast([128, NCH, 64]), ALU.mult,
                   [psb, CONST], [B("LL0")])
                yield
                for k_, (src_ap, srcb, dst, dstb, coff) in enumerate((
                    (lambda h, c: AR[hsl[h], 0, csl(c)], ARb, G[0], "G0", 0),
                    (lambda h, c: vbf[hsl[h], csl(c)], vbfb, Vtok, "Vtok", None),
                    (lambda h, c: BKH[hsl[h], 0, csl(c)], BKHb, Btok, "Btok", None),
                    (lambda h, c: BKH[hsl[h], 1, csl(c)], BKHb, Ktok, "Ktok", None),
                )):
                    ps, psb = nps("Y1")
                    for c in range(NCH):
                        for h in range(2):
                            mm(ps[hsl[h], c * 64:(c + 1) * 64], src_ap(h, c), identB[hsl[h], hsl[h]], True, True,
                               [srcb, CONST], [psb])
                    if coff is None:
                        act(dst[:, :, :], v3(ps[:, :], 64), AF.Copy, [psb], [B(dstb)])
                    else:
                        act(dst[:, :, 0:64], v3(ps[:, :], 64), AF.Copy, [psb], [B(dstb)])
                    yield
                ps, psb = nps("Y1")
                for c in range(NCH):
                    for h in range(2):
                        mm(ps[hsl[h], c * 64:(c + 1) * 64], SCk[hsl[h], c, 0:64], Vtok[hsl[h], c, :], True, True,
                           [B("SCk"), B("Vtok")], [psb])
                cp(G[0][:, :, 64:128], v3(ps[:, :], 64), [psb], [B("G0")])
                yield
                gi = 0
                for lev in range(6):
                    Nk = SCb if lev == 0 else NN[lev % 2]
                    Nkb = B("SCb") if lev == 0 else B(f"NN{lev % 2}")
                    Lk, Lkb = LL[lev % 2], B(f"LL{lev % 2}")
                    pss = []
                    for half in range(2):
                        ps, psb = nps("Y1")
                        pss.append((ps, psb))
                        for cc in range(4):
                            c = half * 4 + cc
                            for h in range(2):
                                mm(ps[hsl[h], cc * 128:(cc + 1) * 128], Nk[hsl[h], c, 0:64], G[gi][hsl[h], c, :], True, True,
                                   [Nkb, B(f"G{gi}")], [psb])
                    nxt = (lev + 1) % 2
                    if lev < 5:
                        psn, psnb = nps("Y1")
                        for c in range(NCH):
                            for h in range(2):
                                mm(psn[hsl[h], c * 64:(c + 1) * 64], Lk[hsl[h], c, :], Nk[hsl[h], c, 0:64], True, True,
                                   [Lkb, Nkb], [psnb])
                    for half in range(2):
                        ps, psb = pss[half]
                        tt(G[1 - gi][:, half * 4:half * 4 + 4, :], v3(ps[:, :], 128),
                           G[gi][:, half * 4:half * 4 + 4, :], ALU.add, [psb, B(f"G{gi}")], [B(f"G{1 - gi}")])
                    gi = 1 - gi
                    if lev < 5:
                        act(NN[nxt][:, :, :], v3(psn[:, :], 64), AF.Copy, [psnb], [B(f"NN{nxt}")])
                    yield
                    if lev < 4:
                        ps, psb = nps("Y1")
                        for c in range(NCH):
                            for h in range(2):
                                mm(ps[hsl[h], c * 64:(c + 1) * 64], Nk[hsl[h], c, 0:64], Lk[hsl[h], c, :], True, True,
                                   [Lkb, Nkb], [psb])
                        act(LL[nxt][:, :, :], v3(ps[:, :], 64), AF.Copy, [psb], [B(f"LL{nxt}")])
                        yield
                TG, TGb = G[gi], B(f"G{gi}")
                ps, psb = nps("Y1")
                for c in range(NCH):
                    for h in range(2):
                        mm(ps[hsl[h], c * 64:(c + 1) * 64], TG[hsl[h], c, 0:64], Btok[hsl[h], c, :], True, True,
                           [TGb, B("Btok")], [psb])
                tt(PT[:, :, :], v3(ps[:, :], 64), DW[:, :, :], ALU.add, [psb, DWb], [PTb])
                yield
                ps, psb = nps("Y1")
                for c in range(NCH):
                    for h in range(2):
                        mm(ps[hsl[h], c * 64:(c + 1) * 64], Btok[hsl[h], c, :], TG[hsl[h], c, 64:128], True, False,
                           [TGb, B("Btok")], [psb])
                        mm(ps[hsl[h], c * 64:(c + 1) * 64], Ktok[hsl[h], c, :], Vtok[hsl[h], c, :], False, True,
                           [B("Ktok"), B("Vtok")], [psb])
                act(Z[:, :, :], v3(ps[:, :], 64), AF.Copy, [psb], [Zb])
                yield
                if own:
                    ps, psb = nps("Y1")
                    for c in range(NCH):
                        for h in range(2):
                            mm(ps[hsl[h], c * 64:(c + 1) * 64], TG[hsl[h], c, 0:64], SCb[hsl[h], c, 64:128], True, True,
                               [TGb, B("SCb")], [psb])
                    tt(QT[:, :, :], v3(ps[:, :], 64), v3(AR[:, 1, :], 64), ALU.add, [psb, ARb], [QTb])
                    yield
                    ps, psb = nps("Y1")
                    for c in range(NCH):
                        for h in range(2):
                            mm(ps[hsl[h], c * 64:(c + 1) * 64], TG[hsl[h], c, 64:128], SCb[hsl[h], c, 64:128], True, False,
                               [TGb, B("SCb")], [psb])
                            mm(ps[hsl[h], c * 64:(c + 1) * 64], Vtok[hsl[h], c, :], SCk[hsl[h], c, 64:128], False, True,
                               [B("Vtok"), B("SCk")], [psb])
                    act(Y0T[:, :, :], v3(ps[:, :], 64), AF.Copy, [psb], [Y0Tb])
                    yield

            def stageY2(it):
                seg, ct = divmod(it, 8)
                own = seg >= NSEG - 2
                osg = seg - (NSEG - 2)
                i2, i3 = it % 2, it % 3
                PT, Z, QT, Y0T = PTs[i2], Zs[i2], QTs[i2], Y0Ts[i2]
                PTb, Zb, QTb, Y0Tb = B(f"PT{i2}"), B(f"Z{i2}"), B(f"QT{i2}"), B(f"Y0T{i2}")
                v_sh, rkr, gT = vks[i3], rkrs[i3], gTs[i3]
                vkb, rkrb, gTb = B(f"vk{i3}"), B(f"rkr{i3}"), B(f"gT{i3}")
                cp(STs[:, 0, :], STp[:, ct, :], [B("STp")], [B("STs")], eng="gpsimd")
                for c in range(NCH):
                    ps, psb = nps("Y2")
                    for h in range(2):
                        mm(ps[hsl[h], 0:64], PT[hsl[h], c, :], STs[hsl[h], c, :], True, True, [PTb, B("STs")], [psb])
                    tt(STs[:, c + 1, :], ps[:, 0:64], Z[:, c, :], ALU.add, [psb, Zb], [B("STs")])
                    yield
                cp(STp[:, ct, :], STs[:, NCH, :], [B("STs")], [B("STp")], eng="gpsimd")
                if not own:
                    return
                ps, psb = nps("Y2")
                for c in range(NCH):
                    for h in range(2):
                        mm(ps[hsl[h], c * 64:(c + 1) * 64], STs[hsl[h], c, :], QT[hsl[h], c, :], True, True,
                           [B("STs"), QTb], [psb])
                tt(yT[:, :], ps[:, :], Y0T[:, :, :].rearrange("p a b -> p (a b)"), ALU.add, [psb, Y0Tb], [B("yT")])
                yield
                ps, psb = nps("Y2")
                mm(ps[:, :], blockones[:, :], yT[:, :], True, True, [CONST, B("yT")], [psb])
                tt(yT[:, :], yT[:, :], ps[:, :], ALU.subtract, [B("yT"), psb], [B("yT")])
                act(t2[:, :], yT[:, :], AF.Square, [B("yT")], [B("t2")])
                yield
                ps, psb = nps("Y2")
                mm(ps[:, :], blockones[:, :], t2[:, :], True, True, [CONST, B("t2")], [psb])
                act(rstd_bc[:, :], ps[:, :], AF.Sqrt, [psb, CONST], [B("rstd_bc")], bias=epst[:, 2:3], scale=1.0)
                recip(rstd_bc[:, :], rstd_bc[:, :], [B("rstd_bc")], [B("rstd_bc")])
                tt(yT[:, :], yT[:, :], rstd_bc[:, :], ALU.mult, [B("yT"), B("rstd_bc")], [B("yT")])
                yield
                ts(yT[:, :], yT[:, :], pc("gn_g", ct), pc("gn_b", ct), ALU.mult, ALU.add, [B("yT"), B("ptab")], [B("yT")])
                ps, psb = nps("Y2")
                mm(ps[:, :], blockones[:, :], rkr[:, :], True, True, [CONST, rkrb], [psb])
                stt(t2[:, :], ps[:, :], 64.0, v_sh[:, :], ALU.mult, ALU.mult, [psb, vkb], [B("t2")])
                yield
                tt(yT[:, :], yT[:, :], t2[:, :], ALU.add, [B("yT"), B("t2")], [B("yT")])
                tt(ybf[:, :], yT[:, :], gT[:, :], ALU.mult, [gTb, B("yT")], [B("ybf")])
                dma("sync", mixscr[:, 8 + ct, osg * SEGN:(osg + 1) * SEGN], ybf[:, :], [B("ybf")], ())
                yield

            NIT = NSEG * 8
            for step in range(NIT + 2):
                gens = []
                if step < NIT:
                    gens.append([stageX(step), 2])
                if 0 <= step - 1 < NIT:
                    gens.append([stageY1(step - 1), 4])
                if 0 <= step - 2 < NIT:
                    gens.append([stageY2(step - 2), 2])
                while gens:
                    for gw in list(gens):
                        for _ in range(gw[1]):
                            try:
                                next(gw[0])
                            except StopIteration:
                                gens.remove(gw)
                                break
            S.barrier()
            S.flush(block)

        es2 = contextlib.ExitStack()
        hT = es2.enter_context(nc.sbuf_tensor("hT", [128, NKT, OWN], F32))
        vT = es2.enter_context(nc.sbuf_tensor("vT", [128, NKT, OWN], BF16))
        with contextlib.ExitStack() as es:
            def T(name, shape, dt=F32):
                return es.enter_context(nc.sbuf_tensor(name, shape, dt))
            mixT = T("mixT", [128, NKT, OWN], BF16)
            xrow2 = [T("xrowb0", [128, D])] * 2
            woutb = [T(f"woutb{i}", [128, NKT, 512], BF16) for i in range(2)]
            dma("sync", mixT[:, :, :], mixscr, (), [B("mixT")])
            if DEBUG:
                for kt in range(NKT):
                    cp(hT[:, kt, :], mixT[:, kt, :], [B("mixT")], [B("hT")])
                dma("sync", dbg_mix, hT[:, :, :], [B("hT")], ())
            for rt in range(8):
                xr = xrow2[rt % 2]
                xb = B("xrowb0")
                r0 = SEQ - OWN + rt * 128
                dma("sync", xr[:, :], xseq[r0:r0 + 128, :], (), [xb])
                for q in range(4):
                    ps, psb = nextps()
                    for i in range(4):
                        kt = q * 4 + i
                        tr(ps[:, i * 128:(i + 1) * 128], xr[:, kt * 128:(kt + 1) * 128], identF[:, :], [xb, CONST], [psb])
                    act(hT[:, q * 4:q * 4 + 4, rt * 128:(rt + 1) * 128], ps[:, :].rearrange("p (a b) -> p a b", b=128), AF.Copy,
                        [psb], [B("hT")])
            for cb in range(4):
                wb_, wbb = woutb[cb % 2], B(f"woutb{cb % 2}")
                dma("gpsimd", wb_[:, :, :], wout_d[cb], (), [wbb])
                for oi in range(4):
                    ot = cb * 4 + oi
                    for tt_ in range(2):
                        ps, psb = nextps()
                        for kt in range(NKT):
                            mm(ps[:, :], wb_[:, kt, oi * 128:(oi + 1) * 128], mixT[:, kt, tt_ * 512:(tt_ + 1) * 512], kt == 0,
                               kt == NKT - 1, [wbb, B("mixT")], [psb])
                        tt(hT[:, ot, tt_ * 512:(tt_ + 1) * 512], hT[:, ot, tt_ * 512:(tt_ + 1) * 512], ps[:, :], ALU.add,
                           [psb, B("hT")], [B("hT")])
            if DEBUG:
                dma("sync", dbg_h, hT[:, :, :], [B("hT")], ())
            S.barrier()
            S.flush(block)

        with contextlib.ExitStack() as es:
            def T(name, shape, dt=F32):
                return es.enter_context(nc.sbuf_tensor(name, shape, dt))
            rw = T("rw_sb", [128, NKT, 36])
            rb3 = T("rb_sb", [128, 1, 36])
            rb = rb3[:, 0, :]
            sqt = T("sqt", [128, 512])
            rstd2 = T("rstd2", [128, OWN])
            rwg = T("rwg", [128, NKT, 36])
            rcol = T("rcol", [128, 1])
            lg = T("lg", [128, 36])
            rt_ = T("rt_", [128, 48])
            m32 = T("m32", [128, 32])
            top8 = T("top8", [128, 8])
            ctok = T("ctok", [128, 8, 32])
            cT = T("cT", [32, OWN])
            selt = T("selt", [32, 128])
            wgb = T("wgb", [128, NKT, DE], BF16)
            wub = T("wub", [128, NKT, DE], BF16)
            wdb = T("wdb", [128, 4, D], BF16)
            hid = T("hid", [128, 4, OWN], BF16)
            sgt = T("sgt", [128, 512])
            crow = T("crow", [128, 512])

            def rms_rstd(eps_col):
                for tt_ in range(2):
                    ps, psb = nextps()
                    for kt in range(NKT):
                        act(sqt[:, :], hT[:, kt, tt_ * 512:(tt_ + 1) * 512], AF.Square, [B("hT")], [B("sqt")])
                        mm(ps[:, :], ones128[:, :], sqt[:, :], kt == 0, kt == NKT - 1, [CONST, B("sqt")], [psb])
                    act(rstd2[:, tt_ * 512:(tt_ + 1) * 512], ps[:, :], AF.Sqrt, [psb, CONST], [B("rstd2")],
                        bias=epst[:, eps_col:eps_col + 1], scale=1.0 / D)
                recip(rstd2[:, :], rstd2[:, :], [B("rstd2")], [B("rstd2")])

            rms_rstd(0)
            dma("sync", rw[:, :, :], rw_d, (), [B("rw")])
            dma("sync", rb3[:, :, :], rb_d.partition_broadcast(128), (), [B("rb")])
            gcol = PCOL["norm_ffn"]
            for kt in range(NKT):
                stt(vT[:, kt, :], hT[:, kt, :], ptab[:, gcol + kt:gcol + kt + 1], rstd2[:, :], ALU.mult, ALU.mult,
                    [B("hT"), B("rstd2"), B("ptab")], [B("vT")])
            for kt in range(NKT):
                ts(rwg[:, kt, :], rw[:, kt, :], ptab[:, gcol + kt:gcol + kt + 1], None, ALU.mult, ALU.bypass,
                   [B("rw"), B("ptab")], [B("rwg")], eng="gpsimd")
            for t8 in range(8):
                tsl = slice(t8 * 128, (t8 + 1) * 128)
                pr, prb = nextps()
                mm(pr[:, 0:2], rstd2[0:1, tsl], ones128[0:1, 0:2], True, True, [B("rstd2"), CONST], [prb])
                act(rcol[:, 0:1], pr[:, 0:1], AF.Copy, [prb], [B("rcol")])
                ps, psb = nextps()
                for kt in range(NKT):
                    mm(ps[:, 0:36], hT[:, kt, tsl], rwg[:, kt, :], kt == 0, kt == NKT - 1, [B("hT"), B("rwg")], [psb])
                stt(lg[:, :], ps[:, 0:36], rcol[:, 0:1], rb, ALU.mult, ALU.add, [psb, B("rcol"), B("rb")], [B("lg")])
                RT = B("rt_")
                S.op("vector", lambda e: e.tensor_reduce(out=rt_[:, 0:1], in_=lg[:, 0:4], axis=mybir.AxisListType.X, op=ALU.max),
                     [B("lg")], [RT])
                ts(rt_[:, 4:8], lg[:, 0:4], rt_[:, 0:1], None, ALU.is_equal, ALU.bypass, [B("lg"), RT], [RT])
                ts(rt_[:, 1:2], rt_[:, 0:1], -1.0, None, ALU.mult, ALU.bypass, [RT], [RT])
                act(rt_[:, 8:12], lg[:, 0:4], AF.Exp, [B("lg"), RT], [RT], bias=rt_[:, 1:2], accum=rt_[:, 2:3])
                recip(rt_[:, 3:4], rt_[:, 2:3], [RT], [RT])
                BIG = 1.0e4
                ts(rt_[:, 12:16], rt_[:, 4:8], BIG, -BIG, ALU.mult, ALU.add, [RT], [RT])
                m3 = m32[:, :].rearrange("p (g e) -> p g e", e=8)
                tt(m3, lg[:, 4:36].rearrange("p (g e) -> p g e", e=8), rt_[:, 4:8].unsqueeze(2).to_broadcast([128, 4, 8]),
                   ALU.mult, [B("lg"), RT], [B("m32")])
                tt(m3, m3, rt_[:, 12:16].unsqueeze(2).to_broadcast([128, 4, 8]), ALU.add, [B("m32"), RT], [B("m32")])
                S.op("vector", lambda e: e.max(out=top8[:, :], in_=m32[:, :]), [B("m32")], [B("top8")])
                tt(rt_[:, 16:17], top8[:, 1:2], top8[:, 0:1], ALU.subtract, [B("top8")], [RT])
                act(rt_[:, 17:18], rt_[:, 16:17], AF.Exp, [RT], [RT])
                ts(rt_[:, 18:19], rt_[:, 17:18], 1.0, None, ALU.add, ALU.bypass, [RT], [RT])
                recip(rt_[:, 19:20], rt_[:, 18:19], [RT], [RT])
                tt(rt_[:, 20:21], rt_[:, 19:20], rt_[:, 3:4], ALU.mult, [RT], [RT])
                tt(rt_[:, 21:22], rt_[:, 20:21], rt_[:, 17:18], ALU.mult, [RT], [RT])
                ts(ctok[:, t8, :], m32[:, :], top8[:, 0:1], rt_[:, 20:21], ALU.is_equal, ALU.mult, [B("m32"), B("top8"), RT],
                   [B("ctok")])
                ts(m32[:, :], m32[:, :], top8[:, 1:2], rt_[:, 21:22], ALU.is_equal, ALU.mult, [B("m32"), B("top8"), RT],
                   [B("m32")])
                tt(ctok[:, t8, :], ctok[:, t8, :], m32[:, :], ALU.add, [B("ctok"), B("m32")], [B("ctok")])
                ps, psb = nextps()
                tr(ps[0:32, 0:128], ctok[:, t8, :], identF[:, :], [B("ctok"), CONST], [psb])
                cp(cT[:, tsl], ps[0:32, 0:128], [psb], [B("cT")])

            for e_ in range(NE):
                dma("gpsimd", wgb[:, :, :], wg_d[e_], (), [B("wgb")])
                dma("gpsimd", wub[:, :, :], wu_d[e_], (), [B("wub")])
                dma("gpsimd", wdb[:, :, :], wd_d[e_], (), [B("wdb")])
                cp(selt[:, :], identF[0:32, e_:e_ + 1].to_broadcast([32, 128]), [CONST], [B("selt")])
                for tt_ in range(2):
                    tsl = slice(tt_ * 512, (tt_ + 1) * 512)
                    pc_, pcb = nextps()
                    mm(pc_[:, :], selt[:, :], cT[:, tsl], True, True, [B("selt"), B("cT")], [pcb])
                    act(crow[:, :], pc_[:, :], AF.Copy, [pcb], [B("crow")])
                    for ht in range(4):
                        pg, pgb = nextps()
                        for kt in range(NKT):
                            mm(pg[:, :], wgb[:, kt, ht * 128:(ht + 1) * 128], vT[:, kt, tsl], kt == 0, kt == NKT - 1,
                               [B("wgb"), B("vT")], [pgb])
                        pu, pub = nextps()
                        for kt in range(NKT):
                            mm(pu[:, :], wub[:, kt, ht * 128:(ht + 1) * 128], vT[:, kt, tsl], kt == 0, kt == NKT - 1,
                               [B("wub"), B("vT")], [pub])
                        act(sgt[:, :], pg[:, :], AF.Silu, [pgb], [B("sgt")])
                        tt(sgt[:, :], sgt[:, :], pu[:, :], ALU.mult, [pub, B("sgt")], [B("sgt")])
                        tt(hid[:, ht, tsl], sgt[:, :], crow[:, :], ALU.mult, [B("crow"), B("sgt")], [B("hid")])
                for tt_ in range(2):
                    tsl = slice(tt_ * 512, (tt_ + 1) * 512)
                    for ot in range(NKT):
                        po, pob = nextps()
                        for k4 in range(4):
                            mm(po[:, :], wdb[:, k4, ot * 128:(ot + 1) * 128], hid[:, k4, tsl], k4 == 0, k4 == 3,
                               [B("wdb"), B("hid")], [pob])
                        tt(hT[:, ot, tsl], hT[:, ot, tsl], po[:, :], ALU.add, [pob, B("hT")], [B("hT")],
                           eng="vector")

            otok = T("otok", [128, D])
            rms_rstd(0)
            gcol = PCOL["norm_final"]
            for kt in range(NKT):
                stt(hT[:, kt, :], hT[:, kt, :], ptab[:, gcol + kt:gcol + kt + 1], rstd2[:, :], ALU.mult, ALU.mult,
                    [B("hT"), B("rstd2"), B("ptab")], [B("hT")])
            for t8 in range(8):
                for q in range(4):
                    ps, psb = nextps()
                    for i in range(4):
                        kt = q * 4 + i
                        tr(ps[:, i * 128:(i + 1) * 128], hT[:, kt, t8 * 128:(t8 + 1) * 128], identF[:, :], [B("hT"), CONST], [psb])
                    if q % 2 == 0:
                        act(otok[:, q * 512:(q + 1) * 512], ps[:, :], AF.Copy, [psb], [B("otok")])
                    else:
                        cp(otok[:, q * 512:(q + 1) * 512], ps[:, :], [psb], [B("otok")])
                dma("sync", out_d[t8 * 128:(t8 + 1) * 128, :], otok[:, :], [B("otok")], ())
            S.barrier()
            S.flush(block)
        es2.close()
    return nc


def _prep_shared(inp):
    f = lambda a: np.ascontiguousarray(np.asarray(a, dtype=np.float32))
    w_in = f(inp["w_in"])[0]
    o = {}

    def pk(cols):
        return np.ascontiguousarray(cols.reshape(NKT, 128, -1).transpose(1, 0, 2))
    wct = np.empty((8, 128, NKT, 384), np.float32)
    wconv = np.empty((8, 128, NKT, 256), np.float32)
    for ct in range(8):
        s = slice(ct * 128, (ct + 1) * 128)
        wct[ct, :, :, 0:128] = pk(w_in[:, 3072:4096][:, s])
        wct[ct, :, :, 128:256] = pk(w_in[:, 4096:5120][:, s])
        wct[ct, :, :, 256:384] = pk(w_in[:, 2048:3072][:, s])
        wconv[ct, :, :, 0:128] = pk(w_in[:, 0:1024][:, s])
        wconv[ct, :, :, 128:256] = pk(w_in[:, 1024:2048][:, s])
    o["wct"] = wct
    o["wconv"] = wconv
    o["wlora"] = pk(w_in[:, 5120:5408])
    o["waup"] = np.ascontiguousarray(np.concatenate([f(inp["w_lora_up"])[0], f(inp["a_lora_up"])[0]], 0))
    gup = f(inp["g_lora_up"])[0]
    o["gup1"] = np.ascontiguousarray(gup[0:128])
    o["gup2"] = np.ascontiguousarray(gup[128:160])
    wout = f(inp["w_out"])[0]
    o["wout"] = np.ascontiguousarray(np.stack([pk(wout[:, cb * 512:(cb + 1) * 512]) for cb in range(4)], 0))
    o["rw"] = pk(np.concatenate([f(inp["router_group_w"])[0], f(inp["router_expert_w"])[0]], 1))
    o["rb"] = np.ascontiguousarray(np.concatenate([f(inp["router_group_b"])[0], f(inp["router_expert_b"])[0]])[None, :])
    wg = f(inp["expert_w_gate"])[0]
    wu = f(inp["expert_w_up"])[0]
    wd = f(inp["expert_w_down"])[0]
    o["wg"] = np.ascontiguousarray(wg.reshape(NE, NKT, 128, DE).transpose(0, 2, 1, 3))
    o["wu"] = np.ascontiguousarray(wu.reshape(NE, NKT, 128, DE).transpose(0, 2, 1, 3))
    o["wd"] = np.ascontiguousarray(wd.reshape(NE, 4, 128, D).transpose(0, 2, 1, 3))
    pt = np.zeros((128, NP), np.float32)
    mu = f(inp["shift_mu"])[0]

    def col8(v):
        return v.reshape(8, 128).T
    names = {"mu_r": mu[0:1024], "mu_k": mu[1024:2048], "mu_v": mu[2048:3072], "w0": f(inp["w0"])[0], "a0": f(inp["a0"])[0],
             "k_k": f(inp["k_k"])[0], "k_a": f(inp["k_a"])[0], "r_k": f(inp["r_k"])[0].reshape(-1),
             "gn_g": f(inp["gn_g"])[0], "gn_b": f(inp["gn_b"])[0]}
    for nm, v in names.items():
        c8 = col8(v)
        for ct in range(8):
            pt[:, PCOL[(nm, ct)]] = c8[:, ct]
    for nm, key in (("conv_b", "conv_b"), ("ln_g", "conv_ln_g"), ("ln_b", "conv_ln_b")):
        c8 = col8(f(inp[key])[0])
        for cc in range(8):
            pt[:, PCOL[(nm, cc)]] = c8[:, cc]
    dw = f(inp["conv_dw"])[0]
    for cc in range(8):
        pt[:, PCOL[("dw", cc)]:PCOL[("dw", cc)] + 31] = dw[:, cc * 128:(cc + 1) * 128].T
    pt[:, PCOL["norm_mix"]:PCOL["norm_mix"] + 16] = f(inp["norm_mix"])[0].reshape(16, 128).T
    pt[:, PCOL["norm_ffn"]:PCOL["norm_ffn"] + 16] = f(inp["norm_ffn"])[0].reshape(16, 128).T
    pt[:, PCOL["norm_final"]:PCOL["norm_final"] + 16] = f(inp["norm_final"]).reshape(16, 128).T
    pt[:, PCOL["mu_wa"]] = mu[3072:3200]
    pt[:, PCOL["mu_g1"]] = mu[3200:3328]
    pt[0:32, PCOL["mu_g2"]] = mu[3328:3360]
    o["ptab"] = pt
    return o


_NC_CACHE = {}


def kernel(**inputs):
    x = np.asarray(inputs["x"], dtype=np.float32)
    shared = _prep_shared(inputs)
    if "nc" not in _NC_CACHE:
        _NC_CACHE["nc"] = build_nc()
    nc = _NC_CACHE["nc"]
    in_maps = []
    for c in range(8):
        b, j = divmod(c, 4)
        xs = np.zeros((SEQ, D), np.float32)
        n = OWN * (j + 1)
        xs[SEQ - n:] = x[b, :n]
        m = dict(shared)
        m["xseq"] = xs
        in_maps.append(m)
    res = run_bass_kernel_spmd(nc, in_maps, core_ids=list(range(8)))
    out = np.empty((2, SEQ, D), np.float32)
    for c in range(8):
        b, j = divmod(c, 4)
        out[b, j * OWN:(j + 1) * OWN] = res.results[c]["out"]
    if DEBUG:
        kernel.last = res
    return out
```

```python
import numpy as np
import concourse.bass as bass
import concourse.mybir as mybir
from concourse.bass_utils import run_bass_kernel_spmd

F32 = mybir.dt.float32
BF16 = mybir.dt.bfloat16
AF = mybir.ActivationFunctionType
ALU = mybir.AluOpType
ENGS = ("tensor", "vector", "scalar", "gpsimd", "sync")

D = 2048
NKT = 16
SEQ = 4096
OWN = 1024
NSEG = 8
SEGN = 512
C = 64
NCH = SEGN // C
NE = 32
DE = 512
DEBUG = False


class Buf:
    __slots__ = ("name", "w", "rs")

    def __init__(self, name):
        self.name = name
        self.w = None
        self.rs = []


class Sched:
    def __init__(self, nc):
        self.nc = nc
        self.rec = {e: [] for e in ENGS}
        self.sem = {}
        self.cnt = {}
        self.seen = {e: {} for e in ENGS}
        for e in ENGS:
            self._mksem(e)

    def _mksem(self, key):
        self.sem[key] = self.nc.alloc_semaphore("s_" + key)
        self.cnt[key] = 0

    def _wait(self, eng, deps):
        for key, n in deps.items():
            if n <= 0 or self.seen[eng].get(key, 0) >= n:
                continue
            if key == "tensor" and eng == "tensor":
                continue
            self.seen[eng][key] = n
            sem = self.sem[key]
            self.rec[eng].append(lambda e, sem=sem, n=n: e.wait_ge(sem, n))

    def _deps(self, reads, writes):
        deps = {}

        def add(kn):
            if kn is not None and deps.get(kn[0], 0) < kn[1]:
                deps[kn[0]] = kn[1]
        for b in reads:
            add(b.w)
        for b in writes:
            add(b.w)
            for r in b.rs:
                add(r)
        return deps

    def _mark(self, me, reads, writes):
        for b in reads:
            b.rs.append(me)
        for b in writes:
            b.w = me
            b.rs = []

    def op(self, eng, fn, reads=(), writes=()):
        self._wait(eng, self._deps(reads, writes))
        self.cnt[eng] += 1
        sem = self.sem[eng]
        self.rec[eng].append(lambda e, fn=fn, sem=sem: fn(e).then_inc(sem, 1))
        self._mark((eng, self.cnt[eng]), reads, writes)

    def dma(self, eng, fn, reads=(), writes=(), key=None):
        if key is None:
            key = "d_" + (writes[0].name if writes else reads[0].name)
        if key not in self.sem:
            self._mksem(key)
        self._wait(eng, self._deps(reads, writes))
        self.cnt[key] += 16
        sem = self.sem[key]
        self.rec[eng].append(lambda e, fn=fn, sem=sem: fn(e).then_inc(sem, 16))
        self._mark((key, self.cnt[key]), reads, writes)

    def barrier(self):
        for e in ENGS:
            self._wait(e, {k: v for k, v in self.cnt.items() if v > 0 and k != e})

    def flush(self, block):
        for e in ENGS:
            lst = self.rec[e]
            if not lst:
                continue
            self.rec[e] = []

            def body(engine, lst=lst):
                for f in lst:
                    f(engine)
            getattr(block, e)(body)


def _ptab_cols():
    cols = {}
    n = 0
    for ct in range(8):
        for nm in ("mu_r", "mu_k", "mu_v", "w0", "a0", "k_k", "k_a", "r_k", "gn_g", "gn_b"):
            cols[(nm, ct)] = n
            n += 1
    for cc in range(8):
        for nm in ("conv_b", "ln_g", "ln_b"):
            cols[(nm, cc)] = n
            n += 1
        cols[("dw", cc)] = n
        n += 31
    for nm in ("norm_mix", "norm_ffn", "norm_final"):
        cols[nm] = n
        n += 16
    for nm in ("mu_wa", "mu_g1", "mu_g2"):
        cols[nm] = n
        n += 1
    return cols, n


PCOL, NP = _ptab_cols()


def build_nc():
    nc = bass.Bass("TRN2", target_bir_lowering=False)
    dt_in = lambda name, shape, dt=F32: nc.dram_tensor(name, shape, dt, kind="ExternalInput").ap()
    xseq = dt_in("xseq", [SEQ, D])
    ptab_d = dt_in("ptab", [128, NP])
    wct_d = dt_in("wct", [8, 128, NKT, 384])
    wlora_d = dt_in("wlora", [128, NKT, 288])
    wconv_d = dt_in("wconv", [8, 128, NKT, 256])
    waup_d = dt_in("waup", [128, 1024])
    gup1_d = dt_in("gup1", [128, 1024])
    gup2_d = dt_in("gup2", [32, 1024])
    wout_d = dt_in("wout", [4, 128, NKT, 512])
    rw_d = dt_in("rw", [128, NKT, 36])
    rb_d = dt_in("rb", [1, 36])
    wg_d = dt_in("wg", [NE, 128, NKT, DE])
    wu_d = dt_in("wu", [NE, 128, NKT, DE])
    wd_d = dt_in("wd", [NE, 128, 4, D])
    out_d = nc.dram_tensor("out", [OWN, D], F32, kind="ExternalOutput").ap()
    if DEBUG:
        dbg_mix = nc.dram_tensor("dbg_mix", [128, 16, OWN], F32, kind="ExternalOutput").ap()
        dbg_h = nc.dram_tensor("dbg_h", [128, 16, OWN], F32, kind="ExternalOutput").ap()

    S = Sched(nc)
    A = nc.alloc_sbuf_tensor
    bufs = {}

    def B(name):
        if name not in bufs:
            bufs[name] = Buf(name)
        return bufs[name]

    def mm(out, lhsT, rhs, start, stop, r, w):
        S.op("tensor", lambda e: e.matmul(out, lhsT=lhsT, rhs=rhs, start=start, stop=stop), r, w)

    def tr(out, in_, ident, r, w):
        S.op("tensor", lambda e: e.transpose(out=out, in_=in_, identity=ident), r, w)

    def act(out, in_, func, r, w, bias=None, scale=None, accum=None):
        kw = {}
        if bias is not None:
            kw["bias"] = bias
        if scale is not None:
            kw["scale"] = scale
        if accum is not None:
            kw["accum_out"] = accum
        S.op("scalar", lambda e: e.activation(out=out, in_=in_, func=func, **kw), r, w)

    def tt(out, a, b, op, r, w, eng="vector"):
        S.op(eng, lambda e: e.tensor_tensor(out=out, in0=a, in1=b, op=op), r, w)

    def ts(out, a, s1, s2, op0, op1, r, w, eng="vector"):
        S.op(eng, lambda e: e.tensor_scalar(out=out, in0=a, scalar1=s1, scalar2=s2, op0=op0, op1=op1), r, w)

    def stt(out, a, s, b, op0, op1, r, w, eng="vector"):
        S.op(eng, lambda e: e.scalar_tensor_tensor(out=out, in0=a, scalar=s, in1=b, op0=op0, op1=op1), r, w)

    def cp(out, in_, r, w, eng="vector"):
        S.op(eng, lambda e: e.tensor_copy(out=out, in_=in_), r, w)

    def recip(out, in_, r, w):
        S.op("vector", lambda e: e.reciprocal(out=out, in_=in_), r, w)

    def memset(ap, val, w, eng="gpsimd"):
        S.op(eng, lambda e: e.memset(ap, val), (), w)

    def dma(eng, out, in_, r, w, key=None):
        S.dma(eng, lambda e: e.dma_start(out=out, in_=in_), r, w, key)

    identF = A("identF", [128, 128], F32)
    identB = A("identB", [128, 128], BF16)
    ones128 = A("ones128", [128, 128], F32)
    blockones = A("blockones", [128, 128], F32)
    ident2 = A("ident2", [128, 64], F32)
    maskS = A("maskS", [128, 128], F32)
    maskL = A("maskL", [128, 64], F32)
    chunkmask = A("chunkmask", [128, SEGN], F32)
    ptab = A("ptab_sb", [128, NP], F32)
    omka = A("omka", [128, 8], F32)
    epst = A("epst", [128, 4], F32)
    PS = [nc.alloc_psum_tensor(f"ps{i}", [128, 512], F32) for i in range(8)]
    PB = [B(f"ps{i}") for i in range(8)]
    CONST = B("const")

    def pc(name, ct=None, n=1):
        c0 = PCOL[(name, ct)] if ct is not None else PCOL[name]
        return ptab[:, c0:c0 + n]

    psi = [0]

    def nextps():
        i = psi[0]
        psi[0] = (i + 1) % 8
        return PS[i], PB[i]

    with nc.Block() as block:
        dma("sync", ptab[:, :], ptab_d, (), [B("ptab")])
        memset(identF[:, :], 0.0, [CONST])
        S.op("gpsimd", lambda e: e.affine_select(out=identF[:, :], in_=identF[:, :], pattern=[[-1, 128]],
                                                 compare_op=ALU.not_equal, fill=1.0, base=0, channel_multiplier=1),
             [CONST], [CONST])
        cp(identB[:, :], identF[:, :], [CONST], [CONST])
        memset(ones128[:, :], 1.0, [CONST])
        memset(blockones[:, :], 0.0, [CONST])
        memset(blockones[0:64, 0:64], 1.0 / 64.0, [CONST])
        memset(blockones[64:128, 64:128], 1.0 / 64.0, [CONST])
        tt(ident2[:, :], identF[:, 0:64], identF[:, 64:128], ALU.add, [CONST], [CONST])
        memset(maskS[:, :], 1.0, [CONST])
        memset(maskL[:, :], 1.0, [CONST])
        for h in range(2):
            hs = slice(h * 64, (h + 1) * 64)
            S.op("gpsimd", lambda e, hs=hs: e.affine_select(out=maskS[hs, 0:64], in_=maskS[hs, 0:64], pattern=[[1, 64]],
                                                            compare_op=ALU.is_gt, fill=0.0, base=0, channel_multiplier=-1),
                 [CONST], [CONST])
            S.op("gpsimd", lambda e, hs=hs: e.affine_select(out=maskS[hs, 64:128], in_=maskS[hs, 64:128], pattern=[[1, 64]],
                                                            compare_op=ALU.is_ge, fill=0.0, base=0, channel_multiplier=-1),
                 [CONST], [CONST])
            S.op("gpsimd", lambda e, hs=hs: e.affine_select(out=maskL[hs, :], in_=maskL[hs, :], pattern=[[-1, 64]],
                                                            compare_op=ALU.is_gt, fill=0.0, base=0, channel_multiplier=1),
                 [CONST], [CONST])
        memset(chunkmask[:, :], 1.0, [CONST])
        memset(chunkmask[:, :].rearrange("p (c t) -> p c t", t=C)[:, :, 0:1], 0.0, [CONST])
        memset(epst[:, 0:1], 1e-6, [CONST])
        memset(epst[:, 1:2], 1e-5, [CONST])
        memset(epst[:, 2:3], 64e-5, [CONST])
        memset(epst[:, 3:4], 0.0, [CONST])
        for ct in range(8):
            ts(omka[:, ct:ct + 1], pc("k_a", ct), -1.0, 1.0, ALU.mult, ALU.add, [B("ptab"), CONST], [CONST])
        S.barrier()
        S.flush(block)

        import contextlib
        mixscr = nc.dram_tensor("mixscr", [128, NKT, OWN], BF16).ap()
        with contextlib.ExitStack() as es:
            def T(name, shape, dt=F32):
                return es.enter_context(nc.sbuf_tensor(name, shape, dt))
            xrows = [T(f"xrow{i}", [128, D]) for i in range(2)]
            ssq = T("ssq", [128, 2])
            uT = T("uT", [128, NKT, SEGN], BF16)
            wcts = [T(f"wct{i}", [128, NKT, 384], BF16) for i in range(2)]
            wlora = T("wlora_sb", [128, NKT, 288], BF16)
            wconv = T("wconv_sb", [128, NKT, 256], BF16)
            waup = T("waup_sb", [128, 1024], BF16)
            gup1 = T("gup1_sb", [128, 1024], BF16)
            gup2 = T("gup2_sb", [32, 1024], BF16)
            raw = T("raw", [128, SEGN + 1])
            dtmp = T("dtmp", [128, SEGN])
            carry = T("carry", [128, 8, 3])
            lcarry = T("lcarry", [128, 3])
            twad = T("twad", [128, SEGN], BF16)
            sg1 = T("sg1", [128, SEGN], BF16)
            sg2 = T("sg2", [32, SEGN], BF16)
            lsh = T("lsh", [128, SEGN])
            k_sh = T("k_sh", [128, SEGN])
            r_sh = T("r_sh", [128, SEGN])
            ls = T("ls", [128, SEGN])
            asig = T("asig", [128, SEGN])
            cl = T("cl", [128, SEGN])
            t0 = T("t0", [128, SEGN])
            e_neg = T("e_neg", [128, SEGN])
            e_pos = T("e_pos", [128, SEGN])
            e_prev = T("e_prev", [128, SEGN])
            e_hat = T("e_hat", [128, SEGN])
            kk = T("kk", [128, SEGN])
            t1 = T("t1", [128, SEGN])
            kmod = T("kmod", [128, SEGN])
            bvec = T("bvec", [128, SEGN])
            ARs = [T(f"AR{i}", [128, 2, SEGN], BF16) for i in range(2)]
            BKs = [T(f"BK{i}", [128, 2, SEGN], BF16) for i in range(2)]
            BKHs = [T(f"BKH{i}", [128, 2, SEGN], BF16) for i in range(2)]
            vbfs = [T(f"vbf{i}", [128, SEGN], BF16) for i in range(2)]
            DWs = [T(f"DW{i}", [128, NCH, 64]) for i in range(2)]
            vks = [T(f"vk{i}", [128, SEGN]) for i in range(3)]
            rkrs = [T(f"rkr{i}", [128, SEGN]) for i in range(3)]
            gTs = [T(f"gT{i}", [128, SEGN]) for i in range(3)]
            SCb = T("SCb", [128, NCH, 128], BF16)
            SCk = T("SCk", [128, NCH, 128], BF16)
            NN = [T(f"NN{i}", [128, NCH, 64], BF16) for i in range(2)]
            LL = [T(f"LL{i}", [128, NCH, 64], BF16) for i in range(2)]
            G = [T(f"G{i}", [128, NCH, 128], BF16) for i in range(2)]
            Vtok = T("Vtok", [128, NCH, 64], BF16)
            Btok = T("Btok", [128, NCH, 64], BF16)
            Ktok = T("Ktok", [128, NCH, 64], BF16)
            PTs = [T(f"PT{i}", [128, NCH, 64], BF16) for i in range(2)]
            Zs = [T(f"Z{i}", [128, NCH, 64]) for i in range(2)]
            QTs = [T(f"QT{i}", [128, NCH, 64], BF16) for i in range(2)]
            Y0Ts = [T(f"Y0T{i}", [128, NCH, 64]) for i in range(2)]
            STs = T("STs", [128, NCH + 1, 64], BF16)
            STp = T("STp", [128, 8, 64], BF16)
            yT = T("yT", [128, SEGN])
            ybf = T("ybf", [128, SEGN], BF16)
            t2 = T("t2", [128, SEGN])
            rstd_bc = T("rstd_bc", [128, SEGN])
            glu = T("glu", [128, 32 + SEGN], BF16)
            gluc = T("gluc", [128, 8, 32], BF16)
            dg = T("dg", [128, 4, 128], BF16)
            conv_out = T("conv_out", [128, 8, SEGN], BF16)
            sq_junk = conv_out[:, 0:4, :]
            onesB = T("onesB", [128, 128], BF16)
            uhalo = T("uhalo", [128, NKT, 32], BF16)
            cbf = ybf
            cmean, crstd, ct1 = e_neg, e_pos, t1

            dma("gpsimd", wlora[:, :, :], wlora_d, (), [B("wlora")])
            dma("gpsimd", waup[:, :], waup_d, (), [B("waup")])
            dma("gpsimd", gup1[:, :], gup1_d, (), [B("gup1")])
            dma("gpsimd", gup2[:, :], gup2_d, (), [B("gup2")])
            memset(carry[:, :, :], 0.0, [B("carry")])
            memset(lcarry[:, :], 0.0, [B("lcarry")])
            memset(STp[:, :, :], 0.0, [B("STp")])
            memset(gluc[:, :, :], 0.0, [B("gluc")])
            cp(onesB[:, :], ones128[:, :], [CONST], [B("onesB")])

            PSETS = {"X": [0, 1], "Y1": [2, 3, 4, 5], "Y2": [6, 7]}
            pidx = {"X": 0, "Y1": 0, "Y2": 0}

            def nps(stage):
                lst = PSETS[stage]
                i = lst[pidx[stage] % len(lst)]
                pidx[stage] += 1
                return PS[i], PB[i]

            hsl = [slice(0, 64), slice(64, 128)]

            def csl(c):
                return slice(c * C, (c + 1) * C)

            def v3(ap, b):
                return ap.rearrange("p (a b) -> p a b", b=b)

            def shift(ps_ap, np_, carry_ap, mu_ap, out_ap, rb, wb, cb):
                act(raw[0:np_, 1:SEGN + 1], ps_ap, AF.Copy, rb, [B("raw")])
                act(raw[0:np_, 0:1], carry_ap, AF.Copy, [cb, B("raw")], [B("raw")])
                tt(dtmp[0:np_, :], raw[0:np_, 0:SEGN], raw[0:np_, 1:SEGN + 1], ALU.subtract, [B("raw")], [B("dtmp")])
                stt(out_ap, dtmp[0:np_, :], mu_ap, raw[0:np_, 1:SEGN + 1], ALU.mult, ALU.add,
                    [B("dtmp"), B("raw"), B("ptab")], wb)
                act(carry_ap, raw[0:np_, SEGN:SEGN + 1], AF.Copy, [B("raw")], [cb])

            def proj(ps, psb, wtile, wb, c0, m, rhsT, rhsb, ncols=SEGN):
                for kt in range(NKT):
                    mm(ps[0:m, 0:ncols], wtile[:, kt, c0:c0 + m], rhsT[:, kt, 0:ncols], kt == 0, kt == NKT - 1,
                       [wb, rhsb], [psb])

            def conv_glu(rhsT, rhsb, ncols, cc, dst_ap, dstb):
                dma("gpsimd", wconv[:, :, :], wconv_d[cc], (), [B("wconv")])
                pv, pvb = nps("X")
                proj(pv, pvb, wconv, B("wconv"), 0, 128, rhsT, rhsb, ncols)
                pg, pgb = nps("X")
                proj(pg, pgb, wconv, B("wconv"), 128, 128, rhsT, rhsb, ncols)
                act(ct1[:, 0:ncols], pg[:, 0:ncols], AF.Sigmoid, [pgb], [B("t1")])
                tt(dst_ap, pv[:, 0:ncols], ct1[:, 0:ncols], ALU.mult, [pvb, B("t1")], dstb)

            def prologue(seg):
                def xload(g):
                    if g < NSEG * 4:
                        dma("sync", xrows[g % 2][:, :], xseq[g * 128:(g + 1) * 128, :], (), [B(f"xrow{g % 2}")])
                if seg == 0:
                    xload(0)
                    xload(1)
                for rt in range(4):
                    g_ = seg * 4 + rt
                    xrow, xb = xrows[g_ % 2], B(f"xrow{g_ % 2}")
                    act(sq_junk, xrow[:, :].rearrange("p (a b) -> p a b", b=SEGN), AF.Square, [xb], [B("conv_all"), B("ssq")], accum=ssq[:, 0:1])
                    act(ssq[:, 1:2], ssq[:, 0:1], AF.Sqrt, [B("ssq"), CONST], [B("ssq")], bias=epst[:, 0:1], scale=1.0 / D)
                    recip(ssq[:, 1:2], ssq[:, 1:2], [B("ssq")], [B("ssq")])
                    ts(xrow[:, :], xrow[:, :], ssq[:, 1:2], None, ALU.mult, ALU.bypass, [xb, B("ssq")], [xb])
                    yield
                    for q in range(4):
                        ps, psb = nps("X")
                        for i in range(4):
                            kt = q * 4 + i
                            tr(ps[:, i * 128:(i + 1) * 128], xrow[:, kt * 128:(kt + 1) * 128], identF[:, :], [xb, CONST], [psb])
                        gcol = PCOL["norm_mix"] + q * 4
                        tt(uT[:, q * 4:q * 4 + 4, rt * 128:(rt + 1) * 128], v3(ps[:, :], 128),
                           ptab[:, gcol:gcol + 4].unsqueeze(2).to_broadcast([128, 4, 128]),
                           ALU.mult, [psb, B("ptab")], [B("uT")])
                        yield
                    xload(g_ + 2)
                ps, psb = nps("X")
                proj(ps, psb, wlora, B("wlora"), 0, 128, uT, B("uT"))
                shift(ps[:, :], 128, lcarry[:, 0:1], pc("mu_wa"), lsh[:, :], [psb], [B("lsh")], B("lcarry"))
                act(twad[0:64, :], lsh[0:64, :], AF.Tanh, [B("lsh")], [B("twad")])
                act(twad[64:128, :], lsh[64:128, :], AF.Copy, [B("lsh")], [B("twad")])
                yield
                ps, psb = nps("X")
                proj(ps, psb, wlora, B("wlora"), 128, 128, uT, B("uT"))
                shift(ps[:, :], 128, lcarry[:, 1:2], pc("mu_g1"), lsh[:, :], [psb], [B("lsh")], B("lcarry"))
                act(sg1[:, :], lsh[:, :], AF.Sigmoid, [B("lsh")], [B("sg1")])
                yield
                ps, psb = nps("X")
                proj(ps, psb, wlora, B("wlora"), 256, 32, uT, B("uT"))
                shift(ps[0:32, :], 32, lcarry[0:32, 2:3], pc("mu_g2")[0:32, :], lsh[0:32, :], [psb], [B("lsh")], B("lcarry"))
                act(sg2[:, :], lsh[0:32, :], AF.Sigmoid, [B("lsh")], [B("sg2")])
                yield

            def conv_branch(seg, own, osg):
                if seg == NSEG - 3:
                    cp(uhalo[:, :, :], uT[:, :, SEGN - 32:SEGN], [B("uT")], [B("uhalo")], eng="gpsimd")
                    for cc in range(8):
                        conv_glu(uhalo, B("uhalo"), 32, cc, gluc[:, cc, :], [B("gluc")])
                        yield
                if not own:
                    return
                for cc in range(8):
                    gb = B("glu")
                    cp(glu[:, 0:32], gluc[:, cc, :], [B("gluc")], [gb], eng="gpsimd")
                    conv_glu(uT, B("uT"), SEGN, cc, glu[:, 32:32 + SEGN], [gb])
                    cp(gluc[:, cc, :], glu[:, SEGN:SEGN + 32], [gb], [B("gluc")], eng="gpsimd")
                    yield
                    dwc = PCOL[("dw", cc)]
                    cob = B("conv_all")
                    ps, psb = nps("X")
                    for j in range(31):
                        dgb = B(f"dg{j % 4}")
                        ts(dg[:, j % 4, :], identB[:, :], ptab[:, dwc + j:dwc + j + 1], None, ALU.mult, ALU.bypass,
                           [CONST, B("ptab")], [dgb])
                        mm(ps[:, :], dg[:, j % 4, :], glu[:, 2 + j:2 + j + SEGN], j == 0, j == 30, [dgb, gb], [psb])
                        if j % 8 == 7:
                            yield
                    act(conv_out[:, cc, :], ps[:, :], AF.Identity, [psb, B("ptab")], [cob], bias=pc("conv_b", cc))
                    yield
                cob = B("conv_all")
                ps, psb = nps("X")
                for cc in range(8):
                    mm(ps[:, :], onesB[:, :], conv_out[:, cc, :], cc == 0, cc == 7, [B("onesB"), cob], [psb])
                act(cmean[:, :], ps[:, :], AF.Copy, [psb], [B("e_neg")], scale=1.0 / 1024.0)
                yield
                ps, psb = nps("X")
                for cc in range(8):
                    tt(kk[:, :], conv_out[:, cc, :], cmean[:, :], ALU.subtract, [cob, B("e_neg")], [B("kk")])
                    act(ct1[:, :], kk[:, :], AF.Square, [B("kk")], [B("t1")])
                    mm(ps[:, :], ones128[:, :], ct1[:, :], cc == 0, cc == 7, [CONST, B("t1")], [psb])
                    yield
                act(crstd[:, :], ps[:, :], AF.Sqrt, [psb, CONST], [B("e_pos")], bias=epst[:, 1:2], scale=1.0 / 1024.0)
                recip(crstd[:, :], crstd[:, :], [B("e_pos")], [B("e_pos")])
                for cc in range(8):
                    tt(kk[:, :], conv_out[:, cc, :], cmean[:, :], ALU.subtract, [cob, B("e_neg")], [B("kk")])
                    tt(ct1[:, :], kk[:, :], crstd[:, :], ALU.mult, [B("kk"), B("e_pos")], [B("t1")])
                    act(cbf[:, :], ct1[:, :], AF.Silu, [B("t1"), B("ptab")], [B("ybf")],
                        bias=pc("ln_b", cc), scale=pc("ln_g", cc))
                    dma("sync", mixscr[:, cc, osg * SEGN:(osg + 1) * SEGN], cbf[:, :], [B("ybf")], ())
                    yield

            K0 = 0.6065306597126334

            def stageX(it):
                seg, ct = divmod(it, 8)
                own = seg >= NSEG - 2
                i2, i3 = it % 2, it % 3
                AR, BK, BKH, vbf, DW = ARs[i2], BKs[i2], BKHs[i2], vbfs[i2], DWs[i2]
                ARb, BKb, BKHb, vbfb, DWb = B(f"AR{i2}"), B(f"BK{i2}"), B(f"BKH{i2}"), B(f"vbf{i2}"), B(f"DW{i2}")
                v_sh, rkr, gT = vks[i3], rkrs[i3], gTs[i3]
                vkb, rkrb, gTb = B(f"vk{i3}"), B(f"rkr{i3}"), B(f"gT{i3}")
                if ct == 0:
                    yield from prologue(seg)
                wct, wtb = wcts[it % 2], B(f"wct{it % 2}")
                if it == 0:
                    dma("gpsimd", wct[:, :, :], wct_d[ct], (), [wtb])
                if it + 1 < NSEG * 8:
                    dma("gpsimd", wcts[(it + 1) % 2][:, :, :], wct_d[(ct + 1) % 8], (), [B(f"wct{(it + 1) % 2}")])
                cols = slice(ct * 128, (ct + 1) * 128)
                ps, psb = nps("X")
                proj(ps, psb, wct, wtb, 0, 128, uT, B("uT"))
                shift(ps[:, :], 128, carry[:, ct, 0:1], pc("mu_k", ct), k_sh[:, :], [psb], [B("k_sh")], B("carry"))
                yield
                ps, psb = nps("X")
                proj(ps, psb, wct, wtb, 128, 128, uT, B("uT"))
                shift(ps[:, :], 128, carry[:, ct, 1:2], pc("mu_v", ct), v_sh[:, :], [psb], [vkb], B("carry"))
                act(vbf[:, :], v_sh[:, :], AF.Copy, [vkb], [vbfb])
                yield
                if own:
                    ps, psb = nps("X")
                    proj(ps, psb, wct, wtb, 256, 128, uT, B("uT"))
                    shift(ps[:, :], 128, carry[:, ct, 2:3], pc("mu_r", ct), r_sh[:, :], [psb], [B("r_sh")], B("carry"))
                    yield
                ps, psb = nps("X")
                mm(ps[:, :], waup[0:64, cols], twad[0:64, :], True, True, [B("waup"), B("twad")], [psb])
                act(ls[:, :], ps[:, :], AF.Sigmoid, [psb, B("ptab")], [B("ls")], bias=pc("w0", ct))
                ps, psb = nps("X")
                mm(ps[:, :], waup[64:128, cols], twad[64:128, :], True, True, [B("waup"), B("twad")], [psb])
                act(asig[:, :], ps[:, :], AF.Sigmoid, [psb, B("ptab")], [B("asig")], bias=pc("a0", ct))
                yield
                S.op("vector", lambda e: e.tensor_tensor_scan(out=cl[:, :], data0=chunkmask[:, :], data1=ls[:, :], initial=0.0,
                                                              op0=ALU.mult, op1=ALU.add),
                     [CONST, B("ls")], [B("cl")])
                act(e_neg[:, :], cl[:, :], AF.Exp, [B("cl")], [B("e_neg")], scale=K0)
                act(e_pos[:, :], cl[:, :], AF.Exp, [B("cl")], [B("e_pos")], scale=-K0)
                tt(t0[:, :], cl[:, :], ls[:, :], ALU.subtract, [B("cl"), B("ls")], [B("t0")], eng="gpsimd")
                yield
                act(e_prev[:, :], t0[:, :], AF.Exp, [B("t0")], [B("e_prev")], scale=-K0)
                cl3 = v3(cl[:, :], C)
                tt(v3(t0[:, :], C), cl3[:, :, C - 1:C].to_broadcast([128, NCH, C]), cl3,
                   ALU.subtract, [B("cl"), B("e_prev")], [B("t0")], eng="gpsimd")
                act(e_hat[:, :], t0[:, :], AF.Exp, [B("t0")], [B("e_hat")], scale=-K0)
                ep3 = v3(e_pos[:, :], C)
                tt(DW[:, :, :], ident2[:, :].unsqueeze(1).to_broadcast([128, NCH, 64]),
                   ep3[:, :, C - 1:C].to_broadcast([128, NCH, 64]), ALU.mult, [CONST, B("e_pos")], [DWb], eng="gpsimd")
                yield
                ts(kk[:, :], k_sh[:, :], pc("k_k", ct), None, ALU.mult, ALU.bypass, [B("k_sh"), B("ptab")], [B("kk")])
                act(t1[:, :], kk[:, :], AF.Square, [B("kk")], [B("t1")])
                ps, psb = nps("X")
                mm(ps[:, :], blockones[:, :], t1[:, :], True, True, [CONST, B("t1")], [psb])
                act(t1[:, :], ps[:, :], AF.Sqrt, [psb], [B("t1")], scale=64.0)
                yield
                ts(t1[:, :], t1[:, :], 1e-12, None, ALU.max, ALU.bypass, [B("t1")], [B("t1")])
                recip(t1[:, :], t1[:, :], [B("t1")], [B("t1")])
                tt(kk[:, :], kk[:, :], t1[:, :], ALU.mult, [B("kk"), B("t1")], [B("kk")])
                yield
                ts(t1[:, :], asig[:, :], pc("k_a", ct), omka[:, ct:ct + 1], ALU.mult, ALU.add,
                   [B("asig"), B("ptab"), CONST], [B("t1")])
                tt(kmod[:, :], k_sh[:, :], t1[:, :], ALU.mult, [B("k_sh"), B("t1")], [B("kmod")])
                tt(bvec[:, :], kk[:, :], asig[:, :], ALU.mult, [B("kk"), B("asig")], [B("bvec")], eng="gpsimd")
                yield
                stt(AR[:, 0, :], kk[:, :], -1.0, e_prev[:, :], ALU.mult, ALU.mult, [B("kk"), B("e_prev")], [ARb])
                tt(BK[:, 0, :], bvec[:, :], e_neg[:, :], ALU.mult, [B("bvec"), B("e_neg")], [BKb], eng="gpsimd")
                tt(BK[:, 1, :], kmod[:, :], e_neg[:, :], ALU.mult, [B("kmod"), B("e_neg")], [BKb])
                yield
                tt(BKH[:, 0, :], bvec[:, :], e_hat[:, :], ALU.mult, [B("bvec"), B("e_hat")], [BKHb], eng="gpsimd")
                tt(BKH[:, 1, :], kmod[:, :], e_hat[:, :], ALU.mult, [B("kmod"), B("e_hat")], [BKHb])
                yield
                if own:
                    tt(AR[:, 1, :], r_sh[:, :], e_pos[:, :], ALU.mult, [B("r_sh"), B("e_pos")], [ARb], eng="gpsimd")
                    stt(rkr[:, :], r_sh[:, :], pc("r_k", ct), kmod[:, :], ALU.mult, ALU.mult,
                        [B("r_sh"), B("kmod"), B("ptab")], [rkrb])
                    ps, psb = nps("X")
                    mm(ps[:, :], gup1[:, cols], sg1[:, :], True, False, [B("gup1"), B("sg1")], [psb])
                    mm(ps[:, :], gup2[:, cols], sg2[:, :], False, True, [B("gup2"), B("sg2")], [psb])
                    act(gT[:, :], ps[:, :], AF.Copy, [psb], [gTb])
                    yield
                if ct == 7:
                    yield from conv_branch(seg, own, seg - (NSEG - 2))

            def stageY1(it):
                seg, ct = divmod(it, 8)
                own = seg >= NSEG - 2
                i2 = it % 2
                AR, BK, BKH, vbf, DW = ARs[i2], BKs[i2], BKHs[i2], vbfs[i2], DWs[i2]
                ARb, BKb, BKHb, vbfb, DWb = B(f"AR{i2}"), B(f"BK{i2}"), B(f"BKH{i2}"), B(f"vbf{i2}"), B(f"DW{i2}")
                PT, Z, QT, Y0T = PTs[i2], Zs[i2], QTs[i2], Y0Ts[i2]
                PTb, Zb, QTb, Y0Tb = B(f"PT{i2}"), B(f"Z{i2}"), B(f"QT{i2}"), B(f"Y0T{i2}")
                ncol = 128 if own else 64
                for which, SCx, SCxb in ((0, SCb, B("SCb")), (1, SCk, B("SCk"))):
                    for half in range(2):
                        ps, psb = nps("Y1")
                        for cc in range(4):
                            c = half * 4 + cc
                            for h in range(2):
                                mm(ps[hsl[h], cc * 128:cc * 128 + ncol], BK[hsl[h], which, csl(c)],
                                   AR[hsl[h], 0:(2 if own else 1), csl(c)], True, True, [BKb, ARb], [psb])
                        tt(SCx[:, half * 4:half * 4 + 4, 0:ncol], v3(ps[:, :], 128)[:, :, 0:ncol],
                           maskS[:, 0:ncol].unsqueeze(1).to_broadcast([128, 4, ncol]), ALU.mult, [psb, CONST], [SCxb])
                        yield
                ps, psb = nps("Y1")
                for c in range(NCH):
                    for h in range(2):
                        mm(ps[hsl[h], c * 64:(c + 1) * 64], AR[hsl[h], 0, csl(c)], BK[hsl[h], 0, csl(c)], True, True, [ARb, BKb], [psb])
                tt(LL[0][:, :, :], v3(ps[:, :], 64), maskL[:, :].unsqueeze(1).to_broadcast([128, NCH, 64]), ALU.mult,
                   [psb, CONST], [B("LL0")])
                yield
                for k_, (src_ap, srcb, dst, dstb, coff) in enumerate((
                    (lambda h, c: AR[hsl[h], 0, csl(c)], ARb, G[0], "G0", 0),
                    (lambda h, c: vbf[hsl[h], csl(c)], vbfb, Vtok, "Vtok", None),
                    (lambda h, c: BKH[hsl[h], 0, csl(c)], BKHb, Btok, "Btok", None),
                    (lambda h, c: BKH[hsl[h], 1, csl(c)], BKHb, Ktok, "Ktok", None),
                )):
                    ps, psb = nps("Y1")
                    for c in range(NCH):
                        for h in range(2):
                            mm(ps[hsl[h], c * 64:(c + 1) * 64], src_ap(h, c), identB[hsl[h], hsl[h]], True, True,
                               [srcb, CONST], [psb])
                    if coff is None:
                        act(dst[:, :, :], v3(ps[:, :], 64), AF.Copy, [psb], [B(dstb)])
                    else:
                        act(dst[:, :, 0:64], v3(ps[:, :], 64), AF.Copy, [psb], [B(dstb)])
                    yield
                ps, psb = nps("Y1")
                for c in range(NCH):
                    for h in range(2):
                        mm(ps[hsl[h], c * 64:(c + 1) * 64], SCk[hsl[h], c, 0:64], Vtok[hsl[h], c, :], True, True,
                           [B("SCk"), B("Vtok")], [psb])
                cp(G[0][:, :, 64:128], v3(ps[:, :], 64), [psb], [B("G0")])
                yield
                gi = 0
                for lev in range(6):
                    Nk = SCb if lev == 0 else NN[lev % 2]
                    Nkb = B("SCb") if lev == 0 else B(f"NN{lev % 2}")
                    Lk, Lkb = LL[lev % 2], B(f"LL{lev % 2}")
                    pss = []
                    for half in range(2):
                        ps, psb = nps("Y1")
                        pss.append((ps, psb))
                        for cc in range(4):
                            c = half * 4 + cc
                            for h in range(2):
                                mm(ps[hsl[h], cc * 128:(cc + 1) * 128], Nk[hsl[h], c, 0:64], G[gi][hsl[h], c, :], True, True,
                                   [Nkb, B(f"G{gi}")], [psb])
                    nxt = (lev + 1) % 2
                    if lev < 5:
                        psn, psnb = nps("Y1")
                        for c in range(NCH):
                            for h in range(2):
                                mm(psn[hsl[h], c * 64:(c + 1) * 64], Lk[hsl[h], c, :], Nk[hsl[h], c, 0:64], True, True,
                                   [Lkb, Nkb], [psnb])
                    for half in range(2):
                        ps, psb = pss[half]
                        tt(G[1 - gi][:, half * 4:half * 4 + 4, :], v3(ps[:, :], 128),
                           G[gi][:, half * 4:half * 4 + 4, :], ALU.add, [psb, B(f"G{gi}")], [B(f"G{1 - gi}")])
                    gi = 1 - gi
                    if lev < 5:
                        act(NN[nxt][:, :, :], v3(psn[:, :], 64), AF.Copy, [psnb], [B(f"NN{nxt}")])
                    yield
                    if lev < 4:
                        ps, psb = nps("Y1")
                        for c in range(NCH):
                            for h in range(2):
                                mm(ps[hsl[h], c * 64:(c + 1) * 64], Nk[hsl[h], c, 0:64], Lk[hsl[h], c, :], True, True,
                                   [Lkb, Nkb], [psb])
                        act(LL[nxt][:, :, :], v3(ps[:, :], 64), AF.Copy, [psb], [B(f"LL{nxt}")])
                        yield
                TG, TGb = G[gi], B(f"G{gi}")
                ps, psb = nps("Y1")
                for c in range(NCH):
                    for h in range(2):
                        mm(ps[hsl[h], c * 64:(c + 1) * 64], TG[hsl[h], c, 0:64], Btok[hsl[h], c, :], True, True,
                           [TGb, B("Btok")], [psb])
                tt(PT[:, :, :], v3(ps[:, :], 64), DW[:, :, :], ALU.add, [psb, DWb], [PTb])
                yield
                ps, psb = nps("Y1")
                for c in range(NCH):
                    for h in range(2):
                        mm(ps[hsl[h], c * 64:(c + 1) * 64], Btok[hsl[h], c, :], TG[hsl[h], c, 64:128], True, False,
                           [TGb, B("Btok")], [psb])
                        mm(ps[hsl[h], c * 64:(c + 1) * 64], Ktok[hsl[h], c, :], Vtok[hsl[h], c, :], False, True,
                           [B("Ktok"), B("Vtok")], [psb])
                act(Z[:, :, :], v3(ps[:, :], 64), AF.Copy, [psb], [Zb])
                yield
                if own:
                    ps, psb = nps("Y1")
                    for c in range(NCH):
                        for h in range(2):
                            mm(ps[hsl[h], c * 64:(c + 1) * 64], TG[hsl[h], c, 0:64], SCb[hsl[h], c, 64:128], True, True,
                               [TGb, B("SCb")], [psb])
                    tt(QT[:, :, :], v3(ps[:, :], 64), v3(AR[:, 1, :], 64), ALU.add, [psb, ARb], [QTb])
                    yield
                    ps, psb = nps("Y1")
                    for c in range(NCH):
                        for h in range(2):
                            mm(ps[hsl[h], c * 64:(c + 1) * 64], TG[hsl[h], c, 64:128], SCb[hsl[h], c, 64:128], True, False,
                               [TGb, B("SCb")], [psb])
                            mm(ps[hsl[h], c * 64:(c + 1) * 64], Vtok[hsl[h], c, :], SCk[hsl[h], c, 64:128], False, True,
                               [B("Vtok"), B("SCk")], [psb])
                    act(Y0T[:, :, :], v3(ps[:, :], 64), AF.Copy, [psb], [Y0Tb])
                    yield

            def stageY2(it):
                seg, ct = divmod(it, 8)
                own = seg >= NSEG - 2
                osg = seg - (NSEG - 2)
                i2, i3 = it % 2, it % 3
                PT, Z, QT, Y0T = PTs[i2], Zs[i2], QTs[i2], Y0Ts[i2]
                PTb, Zb, QTb, Y0Tb = B(f"PT{i2}"), B(f"Z{i2}"), B(f"QT{i2}"), B(f"Y0T{i2}")
                v_sh, rkr, gT = vks[i3], rkrs[i3], gTs[i3]
                vkb, rkrb, gTb = B(f"vk{i3}"), B(f"rkr{i3}"), B(f"gT{i3}")
                cp(STs[:, 0, :], STp[:, ct, :], [B("STp")], [B("STs")], eng="gpsimd")
                for c in range(NCH):
                    ps, psb = nps("Y2")
                    for h in range(2):
                        mm(ps[hsl[h], 0:64], PT[hsl[h], c, :], STs[hsl[h], c, :], True, True, [PTb, B("STs")], [psb])
                    tt(STs[:, c + 1, :], ps[:, 0:64], Z[:, c, :], ALU.add, [psb, Zb], [B("STs")])
                    yield
                cp(STp[:, ct, :], STs[:, NCH, :], [B("STs")], [B("STp")], eng="gpsimd")
                if not own:
                    return
                ps, psb = nps("Y2")
                for c in range(NCH):
                    for h in range(2):
                        mm(ps[hsl[h], c * 64:(c + 1) * 64], STs[hsl[h], c, :], QT[hsl[h], c, :], True, True,
                           [B("STs"), QTb], [psb])
                tt(yT[:, :], ps[:, :], Y0T[:, :, :].rearrange("p a b -> p (a b)"), ALU.add, [psb, Y0Tb], [B("yT")])
                yield
                ps, psb = nps("Y2")
                mm(ps[:, :], blockones[:, :], yT[:, :], True, True, [CONST, B("yT")], [psb])
                tt(yT[:, :], yT[:, :], ps[:, :], ALU.subtract, [B("yT"), psb], [B("yT")])
                act(t2[:, :], yT[:, :], AF.Square, [B("yT")], [B("t2")])
                yield
                ps, psb = nps("Y2")
                mm(ps[:, :], blockones[:, :], t2[:, :], True, True, [CONST, B("t2")], [psb])
                act(rstd_bc[:, :], ps[:, :], AF.Sqrt, [psb, CONST], [B("rstd_bc")], bias=epst[:, 2:3], scale=1.0)
                recip(rstd_bc[:, :], rstd_bc[:, :], [B("rstd_bc")], [B("rstd_bc")])
                tt(yT[:, :], yT[:, :], rstd_bc[:, :], ALU.mult, [B("yT"), B("rstd_bc")], [B("yT")])
                yield
                ts(yT[:, :], yT[:, :], pc("gn_g", ct), pc("gn_b", ct), ALU.mult, ALU.add, [B("yT"), B("ptab")], [B("yT")])
                ps, psb = nps("Y2")
                mm(ps[:, :], blockones[:, :], rkr[:, :], True, True, [CONST, rkrb], [psb])
                stt(t2[:, :], ps[:, :], 64.0, v_sh[:, :], ALU.mult, ALU.mult, [psb, vkb], [B("t2")])
                yield
                tt(yT[:, :], yT[:, :], t2[:, :], ALU.add, [B("yT"), B("t2")], [B("yT")])
                tt(ybf[:, :], yT[:, :], gT[:, :], ALU.mult, [gTb, B("yT")], [B("ybf")])
                dma("sync", mixscr[:, 8 + ct, osg * SEGN:(osg + 1) * SEGN], ybf[:, :], [B("ybf")], ())
                yield

            NIT = NSEG * 8
            for step in range(NIT + 2):
                gens = []
                if step < NIT:
                    gens.append([stageX(step), 1])
                if 0 <= step - 1 < NIT:
                    gens.append([stageY1(step - 1), 2])
                if 0 <= step - 2 < NIT:
                    gens.append([stageY2(step - 2), 1])
                while gens:
                    for gw in list(gens):
                        for _ in range(gw[1]):
                            try:
                                next(gw[0])
                            except StopIteration:
                                gens.remove(gw)
                                break
            S.barrier()
            S.flush(block)

        es2 = contextlib.ExitStack()
        hT = es2.enter_context(nc.sbuf_tensor("hT", [128, NKT, OWN], F32))
        vT = es2.enter_context(nc.sbuf_tensor("vT", [128, NKT, OWN], BF16))
        with contextlib.ExitStack() as es:
            def T(name, shape, dt=F32):
                return es.enter_context(nc.sbuf_tensor(name, shape, dt))
            mixT = T("mixT", [128, NKT, OWN], BF16)
            xrow2 = [T("xrowb0", [128, D])] * 2
            woutb = [T(f"woutb{i}", [128, NKT, 512], BF16) for i in range(2)]
            dma("sync", mixT[:, :, :], mixscr, (), [B("mixT")])
            if DEBUG:
                for kt in range(NKT):
                    cp(hT[:, kt, :], mixT[:, kt, :], [B("mixT")], [B("hT")])
                dma("sync", dbg_mix, hT[:, :, :], [B("hT")], ())
            for rt in range(8):
                xr = xrow2[rt % 2]
                xb = B("xrowb0")
                r0 = SEQ - OWN + rt * 128
                dma("sync", xr[:, :], xseq[r0:r0 + 128, :], (), [xb])
                for q in range(4):
                    ps, psb = nextps()
                    for i in range(4):
                        kt = q * 4 + i
                        tr(ps[:, i * 128:(i + 1) * 128], xr[:, kt * 128:(kt + 1) * 128], identF[:, :], [xb, CONST], [psb])
                    act(hT[:, q * 4:q * 4 + 4, rt * 128:(rt + 1) * 128], ps[:, :].rearrange("p (a b) -> p a b", b=128), AF.Copy,
                        [psb], [B("hT")])
            for cb in range(4):
                wb_, wbb = woutb[cb % 2], B(f"woutb{cb % 2}")
                dma("gpsimd", wb_[:, :, :], wout_d[cb], (), [wbb])
                for oi in range(4):
                    ot = cb * 4 + oi
                    for tt_ in range(2):
                        ps, psb = nextps()
                        for kt in range(NKT):
                            mm(ps[:, :], wb_[:, kt, oi * 128:(oi + 1) * 128], mixT[:, kt, tt_ * 512:(tt_ + 1) * 512], kt == 0,
                               kt == NKT - 1, [wbb, B("mixT")], [psb])
                        tt(hT[:, ot, tt_ * 512:(tt_ + 1) * 512], hT[:, ot, tt_ * 512:(tt_ + 1) * 512], ps[:, :], ALU.add,
                           [psb, B("hT")], [B("hT")])
            if DEBUG:
                dma("sync", dbg_h, hT[:, :, :], [B("hT")], ())
            S.barrier()
            S.flush(block)

        with contextlib.ExitStack() as es:
            def T(name, shape, dt=F32):
                return es.enter_context(nc.sbuf_tensor(name, shape, dt))
            rw = T("rw_sb", [128, NKT, 36])
            rb3 = T("rb_sb", [128, 1, 36])
            rb = rb3[:, 0, :]
            sqt = T("sqt", [128, 512])
            rstd2 = T("rstd2", [128, OWN])
            rwg = T("rwg", [128, NKT, 36])
            rcol = T("rcol", [128, 1])
            lg = T("lg", [128, 36])
            rt_ = T("rt_", [128, 48])
            m32 = T("m32", [128, 32])
            top8 = T("top8", [128, 8])
            ctok = T("ctok", [128, 8, 32])
            cT = T("cT", [32, OWN])
            selt = T("selt", [32, 128])
            wgb = T("wgb", [128, NKT, DE], BF16)
            wub = T("wub", [128, NKT, DE], BF16)
            wdb = T("wdb", [128, 4, D], BF16)
            hid = T("hid", [128, 4, OWN], BF16)
            sgt = T("sgt", [128, 512])
            crow = T("crow", [128, 512])

            def rms_rstd(eps_col):
                for tt_ in range(2):
                    ps, psb = nextps()
                    for kt in range(NKT):
                        act(sqt[:, :], hT[:, kt, tt_ * 512:(tt_ + 1) * 512], AF.Square, [B("hT")], [B("sqt")])
                        mm(ps[:, :], ones128[:, :], sqt[:, :], kt == 0, kt == NKT - 1, [CONST, B("sqt")], [psb])
                    act(rstd2[:, tt_ * 512:(tt_ + 1) * 512], ps[:, :], AF.Sqrt, [psb, CONST], [B("rstd2")],
                        bias=epst[:, eps_col:eps_col + 1], scale=1.0 / D)
                recip(rstd2[:, :], rstd2[:, :], [B("rstd2")], [B("rstd2")])

            rms_rstd(0)
            dma("sync", rw[:, :, :], rw_d, (), [B("rw")])
            dma("sync", rb3[:, :, :], rb_d.partition_broadcast(128), (), [B("rb")])
            gcol = PCOL["norm_ffn"]
            for kt in range(NKT):
                stt(vT[:, kt, :], hT[:, kt, :], ptab[:, gcol + kt:gcol + kt + 1], rstd2[:, :], ALU.mult, ALU.mult,
                    [B("hT"), B("rstd2"), B("ptab")], [B("vT")])
            for kt in range(NKT):
                ts(rwg[:, kt, :], rw[:, kt, :], ptab[:, gcol + kt:gcol + kt + 1], None, ALU.mult, ALU.bypass,
                   [B("rw"), B("ptab")], [B("rwg")], eng="gpsimd")
            for t8 in range(8):
                tsl = slice(t8 * 128, (t8 + 1) * 128)
                pr, prb = nextps()
                mm(pr[:, 0:2], rstd2[0:1, tsl], ones128[0:1, 0:2], True, True, [B("rstd2"), CONST], [prb])
                act(rcol[:, 0:1], pr[:, 0:1], AF.Copy, [prb], [B("rcol")])
                ps, psb = nextps()
                for kt in range(NKT):
                    mm(ps[:, 0:36], hT[:, kt, tsl], rwg[:, kt, :], kt == 0, kt == NKT - 1, [B("hT"), B("rwg")], [psb])
                stt(lg[:, :], ps[:, 0:36], rcol[:, 0:1], rb, ALU.mult, ALU.add, [psb, B("rcol"), B("rb")], [B("lg")])
                RT = B("rt_")
                S.op("vector", lambda e: e.tensor_reduce(out=rt_[:, 0:1], in_=lg[:, 0:4], axis=mybir.AxisListType.X, op=ALU.max),
                     [B("lg")], [RT])
                ts(rt_[:, 4:8], lg[:, 0:4], rt_[:, 0:1], None, ALU.is_equal, ALU.bypass, [B("lg"), RT], [RT])
                ts(rt_[:, 1:2], rt_[:, 0:1], -1.0, None, ALU.mult, ALU.bypass, [RT], [RT])
                act(rt_[:, 8:12], lg[:, 0:4], AF.Exp, [B("lg"), RT], [RT], bias=rt_[:, 1:2], accum=rt_[:, 2:3])
                recip(rt_[:, 3:4], rt_[:, 2:3], [RT], [RT])
                BIG = 1.0e4
                ts(rt_[:, 12:16], rt_[:, 4:8], BIG, -BIG, ALU.mult, ALU.add, [RT], [RT])
                m3 = m32[:, :].rearrange("p (g e) -> p g e", e=8)
                tt(m3, lg[:, 4:36].rearrange("p (g e) -> p g e", e=8), rt_[:, 4:8].unsqueeze(2).to_broadcast([128, 4, 8]),
                   ALU.mult, [B("lg"), RT], [B("m32")])
                tt(m3, m3, rt_[:, 12:16].unsqueeze(2).to_broadcast([128, 4, 8]), ALU.add, [B("m32"), RT], [B("m32")])
                S.op("vector", lambda e: e.max(out=top8[:, :], in_=m32[:, :]), [B("m32")], [B("top8")])
                tt(rt_[:, 16:17], top8[:, 1:2], top8[:, 0:1], ALU.subtract, [B("top8")], [RT])
                act(rt_[:, 17:18], rt_[:, 16:17], AF.Exp, [RT], [RT])
                ts(rt_[:, 18:19], rt_[:, 17:18], 1.0, None, ALU.add, ALU.bypass, [RT], [RT])
                recip(rt_[:, 19:20], rt_[:, 18:19], [RT], [RT])
                tt(rt_[:, 20:21], rt_[:, 19:20], rt_[:, 3:4], ALU.mult, [RT], [RT])
                tt(rt_[:, 21:22], rt_[:, 20:21], rt_[:, 17:18], ALU.mult, [RT], [RT])
                ts(ctok[:, t8, :], m32[:, :], top8[:, 0:1], rt_[:, 20:21], ALU.is_equal, ALU.mult, [B("m32"), B("top8"), RT],
                   [B("ctok")])
                ts(m32[:, :], m32[:, :], top8[:, 1:2], rt_[:, 21:22], ALU.is_equal, ALU.mult, [B("m32"), B("top8"), RT],
                   [B("m32")])
                tt(ctok[:, t8, :], ctok[:, t8, :], m32[:, :], ALU.add, [B("ctok"), B("m32")], [B("ctok")])
                ps, psb = nextps()
                tr(ps[0:32, 0:128], ctok[:, t8, :], identF[:, :], [B("ctok"), CONST], [psb])
                cp(cT[:, tsl], ps[0:32, 0:128], [psb], [B("cT")])

            for e_ in range(NE):
                dma("gpsimd", wgb[:, :, :], wg_d[e_], (), [B("wgb")])
                dma("gpsimd", wub[:, :, :], wu_d[e_], (), [B("wub")])
                dma("gpsimd", wdb[:, :, :], wd_d[e_], (), [B("wdb")])
                cp(selt[:, :], identF[0:32, e_:e_ + 1].to_broadcast([32, 128]), [CONST], [B("selt")])
                for tt_ in range(2):
                    tsl = slice(tt_ * 512, (tt_ + 1) * 512)
                    pc_, pcb = nextps()
                    mm(pc_[:, :], selt[:, :], cT[:, tsl], True, True, [B("selt"), B("cT")], [pcb])
                    act(crow[:, :], pc_[:, :], AF.Copy, [pcb], [B("crow")])
                    for ht in range(4):
                        pg, pgb = nextps()
                        for kt in range(NKT):
                            mm(pg[:, :], wgb[:, kt, ht * 128:(ht + 1) * 128], vT[:, kt, tsl], kt == 0, kt == NKT - 1,
                               [B("wgb"), B("vT")], [pgb])
                        pu, pub = nextps()
                        for kt in range(NKT):
                            mm(pu[:, :], wub[:, kt, ht * 128:(ht + 1) * 128], vT[:, kt, tsl], kt == 0, kt == NKT - 1,
                               [B("wub"), B("vT")], [pub])
                        act(sgt[:, :], pg[:, :], AF.Silu, [pgb], [B("sgt")])
                        tt(sgt[:, :], sgt[:, :], pu[:, :], ALU.mult, [pub, B("sgt")], [B("sgt")])
                        tt(hid[:, ht, tsl], sgt[:, :], crow[:, :], ALU.mult, [B("crow"), B("sgt")], [B("hid")])
                for tt_ in range(2):
                    tsl = slice(tt_ * 512, (tt_ + 1) * 512)
                    for ot in range(NKT):
                        po, pob = nextps()
                        for k4 in range(4):
                            mm(po[:, :], wdb[:, k4, ot * 128:(ot + 1) * 128], hid[:, k4, tsl], k4 == 0, k4 == 3,
                               [B("wdb"), B("hid")], [pob])
                        tt(hT[:, ot, tsl], hT[:, ot, tsl], po[:, :], ALU.add, [pob, B("hT")], [B("hT")],
                           eng="vector")

            otok = T("otok", [128, D])
            rms_rstd(0)
            gcol = PCOL["norm_final"]
            for kt in range(NKT):
                stt(hT[:, kt, :], hT[:, kt, :], ptab[:, gcol + kt:gcol + kt + 1], rstd2[:, :], ALU.mult, ALU.mult,
                    [B("hT"), B("rstd2"), B("ptab")], [B("hT")])
            for t8 in range(8):
                for q in range(4):
                    ps, psb = nextps()
                    for i in range(4):
                        kt = q * 4 + i
                        tr(ps[:, i * 128:(i + 1) * 128], hT[:, kt, t8 * 128:(t8 + 1) * 128], identF[:, :], [B("hT"), CONST], [psb])
                    if q % 2 == 0:
                        act(otok[:, q * 512:(q + 1) * 512], ps[:, :], AF.Copy, [psb], [B("otok")])
                    else:
                        cp(otok[:, q * 512:(q + 1) * 512], ps[:, :], [psb], [B("otok")])
                dma("sync", out_d[t8 * 128:(t8 + 1) * 128, :], otok[:, :], [B("otok")], ())
            S.barrier()
            S.flush(block)
        es2.close()
    return nc


def _prep_shared(inp):
    f = lambda a: np.ascontiguousarray(np.asarray(a, dtype=np.float32))
    w_in = f(inp["w_in"])[0]
    o = {}

    def pk(cols):
        return np.ascontiguousarray(cols.reshape(NKT, 128, -1).transpose(1, 0, 2))
    wct = np.empty((8, 128, NKT, 384), np.float32)
    wconv = np.empty((8, 128, NKT, 256), np.float32)
    for ct in range(8):
        s = slice(ct * 128, (ct + 1) * 128)
        wct[ct, :, :, 0:128] = pk(w_in[:, 3072:4096][:, s])
        wct[ct, :, :, 128:256] = pk(w_in[:, 4096:5120][:, s])
        wct[ct, :, :, 256:384] = pk(w_in[:, 2048:3072][:, s])
        wconv[ct, :, :, 0:128] = pk(w_in[:, 0:1024][:, s])
        wconv[ct, :, :, 128:256] = pk(w_in[:, 1024:2048][:, s])
    o["wct"] = wct
    o["wconv"] = wconv
    o["wlora"] = pk(w_in[:, 5120:5408])
    o["waup"] = np.ascontiguousarray(np.concatenate([f(inp["w_lora_up"])[0], f(inp["a_lora_up"])[0]], 0))
    gup = f(inp["g_lora_up"])[0]
    o["gup1"] = np.ascontiguousarray(gup[0:128])
    o["gup2"] = np.ascontiguousarray(gup[128:160])
    wout = f(inp["w_out"])[0]
    o["wout"] = np.ascontiguousarray(np.stack([pk(wout[:, cb * 512:(cb + 1) * 512]) for cb in range(4)], 0))
    o["rw"] = pk(np.concatenate([f(inp["router_group_w"])[0], f(inp["router_expert_w"])[0]], 1))
    o["rb"] = np.ascontiguousarray(np.concatenate([f(inp["router_group_b"])[0], f(inp["router_expert_b"])[0]])[None, :])
    wg = f(inp["expert_w_gate"])[0]
    wu = f(inp["expert_w_up"])[0]
    wd = f(inp["expert_w_down"])[0]
    o["wg"] = np.ascontiguousarray(wg.reshape(NE, NKT, 128, DE).transpose(0, 2, 1, 3))
    o["wu"] = np.ascontiguousarray(wu.reshape(NE, NKT, 128, DE).transpose(0, 2, 1, 3))
    o["wd"] = np.ascontiguousarray(wd.reshape(NE, 4, 128, D).transpose(0, 2, 1, 3))
    pt = np.zeros((128, NP), np.float32)
    mu = f(inp["shift_mu"])[0]

    def col8(v):
        return v.reshape(8, 128).T
    names = {"mu_r": mu[0:1024], "mu_k": mu[1024:2048], "mu_v": mu[2048:3072], "w0": f(inp["w0"])[0], "a0": f(inp["a0"])[0],
             "k_k": f(inp["k_k"])[0], "k_a": f(inp["k_a"])[0], "r_k": f(inp["r_k"])[0].reshape(-1),
             "gn_g": f(inp["gn_g"])[0], "gn_b": f(inp["gn_b"])[0]}
    for nm, v in names.items():
        c8 = col8(v)
        for ct in range(8):
            pt[:, PCOL[(nm, ct)]] = c8[:, ct]
    for nm, key in (("conv_b", "conv_b"), ("ln_g", "conv_ln_g"), ("ln_b", "conv_ln_b")):
        c8 = col8(f(inp[key])[0])
        for cc in range(8):
            pt[:, PCOL[(nm, cc)]] = c8[:, cc]
    dw = f(inp["conv_dw"])[0]
    for cc in range(8):
        pt[:, PCOL[("dw", cc)]:PCOL[("dw", cc)] + 31] = dw[:, cc * 128:(cc + 1) * 128].T
    pt[:, PCOL["norm_mix"]:PCOL["norm_mix"] + 16] = f(inp["norm_mix"])[0].reshape(16, 128).T
    pt[:, PCOL["norm_ffn"]:PCOL["norm_ffn"] + 16] = f(inp["norm_ffn"])[0].reshape(16, 128).T
    pt[:, PCOL["norm_final"]:PCOL["norm_final"] + 16] = f(inp["norm_final"]).reshape(16, 128).T
    pt[:, PCOL["mu_wa"]] = mu[3072:3200]
    pt[:, PCOL["mu_g1"]] = mu[3200:3328]
    pt[0:32, PCOL["mu_g2"]] = mu[3328:3360]
    o["ptab"] = pt
    return o


_NC_CACHE = {}


def kernel(**inputs):
    x = np.asarray(inputs["x"], dtype=np.float32)
    shared = _prep_shared(inputs)
    if "nc" not in _NC_CACHE:
        _NC_CACHE["nc"] = build_nc()
    nc = _NC_CACHE["nc"]
    in_maps = []
    for c in range(8):
        b, j = divmod(c, 4)
        xs = np.zeros((SEQ, D), np.float32)
        n = OWN * (j + 1)
        xs[SEQ - n:] = x[b, :n]
        m = dict(shared)
        m["xseq"] = xs
        in_maps.append(m)
    res = run_bass_kernel_spmd(nc, in_maps, core_ids=list(range(8)))
    out = np.empty((2, SEQ, D), np.float32)
    for c in range(8):
        b, j = divmod(c, 4)
        out[b, j * OWN:(j + 1) * OWN] = res.results[c]["out"]
    if DEBUG:
        kernel.last = res
    return out
```
